# Optimizing a Trainium2 kernel written in Bass

```python
import jax, jax.numpy as jnp
from jax import lax
import numpy as np

D_MODEL = 1024
BATCH = 8
SEQ = 2048
DEPTH = 1

CHUNK = 64
N_META = 16
CONV_DIM = 1024
CONV_WIDTH = 31
RET_HEADS = 4
RET_QK_DIM = 256
RET_V_DIM = 512
ROPE_BASE = 10000.0
N_GROUPS = 4
EXPERTS_PER_GROUP = 4
N_EXPERTS = N_GROUPS * EXPERTS_PER_GROUP
D_EXPERT = 512
TOP_K_INNER = 2
EPS = 1e-6
IN_SPLITS = (2 * CONV_DIM, RET_HEADS * RET_QK_DIM, RET_HEADS * RET_QK_DIM,
             RET_HEADS * RET_V_DIM, RET_HEADS * RET_V_DIM, 2 * D_MODEL)
D_IN = sum(IN_SPLITS)

kernel_name = "hybrid_conv_retention_hmoe_block"


def rmsnorm(x, g):
    xf = x.astype(jnp.float32)
    y = xf * lax.rsqrt(jnp.mean(xf * xf, axis=-1, keepdims=True) + EPS)
    return (y * g.astype(jnp.float32)).astype(x.dtype)


def layernorm(x, g, b):
    xf = x.astype(jnp.float32)
    mu = jnp.mean(xf, axis=-1, keepdims=True)
    var = jnp.mean(jnp.square(xf - mu), axis=-1, keepdims=True)
    y = (xf - mu) * lax.rsqrt(var + EPS)
    return (y * g.astype(jnp.float32) + b.astype(jnp.float32)).astype(x.dtype)


def conv_module(u, w_dw, b_dw, ln_g, ln_b, w_pw):
    a, gate = jnp.split(u, 2, axis=-1)
    h = a * jax.nn.sigmoid(gate)
    h = jnp.pad(h, ((0, 0), (CONV_WIDTH - 1, 0), (0, 0)))
    h = lax.conv_general_dilated(h, w_dw[:, None, :].astype(h.dtype), window_strides=(1,),
                                 padding='VALID', dimension_numbers=('NWC', 'WIO', 'NWC'),
                                 feature_group_count=CONV_DIM) + b_dw
    h = jax.nn.silu(layernorm(h, ln_g, ln_b))
    return h @ w_pw


def rotary(x, pos):
    half = x.shape[-1] // 2
    inv = ROPE_BASE ** (-jnp.arange(half, dtype=jnp.float32) / half)
    ang = pos[:, None] * inv[None, :]
    cos, sin = jnp.cos(ang)[:, None, :], jnp.sin(ang)[:, None, :]
    x1, x2 = x[..., :half], x[..., half:]
    return jnp.concatenate([x1 * cos - x2 * sin, x2 * cos + x1 * sin], axis=-1)


def retention(q, k, v, gn_g):
    B, L = q.shape[0], q.shape[1]
    pos = jnp.arange(L, dtype=jnp.float32)
    q = rotary(q.astype(jnp.float32).reshape(B, L, RET_HEADS, RET_QK_DIM), pos)
    k = rotary(k.astype(jnp.float32).reshape(B, L, RET_HEADS, RET_QK_DIM), pos) * (RET_QK_DIM ** -0.5)
    v = v.astype(jnp.float32).reshape(B, L, RET_HEADS, RET_V_DIM)
    pad_front = (CHUNK - N_META % CHUNK) % CHUNK
    padw = ((0, 0), (pad_front, 0), (0, 0), (0, 0))
    q, k, v = jnp.pad(q, padw), jnp.pad(k, padw), jnp.pad(v, padw)
    Lp = L + pad_front
    nc = Lp // CHUNK

    def to_chunks(t):
        return t.reshape(B, nc, CHUNK, RET_HEADS, t.shape[-1]).transpose(1, 0, 3, 2, 4)

    qc, kc, vc = to_chunks(q), to_chunks(k), to_chunks(v)
    log_gamma = jnp.log(1.0 - 2.0 ** (-5.0 - jnp.arange(RET_HEADS, dtype=jnp.float32)))
    idx = jnp.arange(CHUNK, dtype=jnp.float32)
    intra_decay = jnp.exp(log_gamma[:, None, None] * jnp.abs(idx[:, None] - idx[None, :]))
    q_decay = jnp.exp(log_gamma[:, None] * (idx + 1.0))[:, :, None]
    k_decay = jnp.exp(log_gamma[:, None] * (CHUNK - 1.0 - idx))[:, :, None]
    chunk_decay = jnp.exp(log_gamma * CHUNK)[:, None, None]

    def step(state, inp):
        qi, ki, vi = inp
        scores = jnp.einsum('bhnd,bhmd->bhnm', qi, ki) * intra_decay
        out = (jnp.einsum('bhnm,bhme->bhne', scores, vi)
               + jnp.einsum('bhnd,bhde->bhne', qi, state) * q_decay)
        state = state * chunk_decay + jnp.einsum('bhmd,bhme->bhde', ki * k_decay, vi)
        return state, out

    s0 = jnp.zeros((B, RET_HEADS, RET_QK_DIM, RET_V_DIM), jnp.float32)
    _, o = lax.scan(step, s0, (qc, kc, vc))
    o = o.transpose(1, 0, 3, 2, 4).reshape(B, Lp, RET_HEADS, RET_V_DIM)[:, pad_front:]
    mu = jnp.mean(o, axis=-1, keepdims=True)
    var = jnp.mean(jnp.square(o - mu), axis=-1, keepdims=True)
    o = ((o - mu) * lax.rsqrt(var + EPS)).reshape(B, L, RET_HEADS * RET_V_DIM)
    return o * gn_g.astype(jnp.float32)


def hier_moe(h, w_group, b_group, w_exp_r, b_exp_r, w_gate, w_up, w_down):
    B, L, D = h.shape
    xt = h.reshape(B * L, D)
    glog = (xt @ w_group + b_group).astype(jnp.float32)
    gprob = jax.nn.softmax(glog, axis=-1)
    top_group = jnp.argmax(gprob, axis=-1)
    p_group = jnp.take_along_axis(gprob, top_group[:, None], axis=-1)
    elog = (xt @ w_exp_r + b_exp_r).astype(jnp.float32).reshape(-1, N_GROUPS, EXPERTS_PER_GROUP)
    sel = jnp.take_along_axis(elog, top_group[:, None, None], axis=1)[:, 0]
    vals, eidx = lax.top_k(sel, TOP_K_INNER)
    weights = p_group * jax.nn.softmax(vals, axis=-1)
    gid = top_group[:, None] * EXPERTS_PER_GROUP + eidx
    comb = jnp.einsum('tk,tke->te', weights,
                      jax.nn.one_hot(gid, N_EXPERTS, dtype=jnp.float32)).astype(xt.dtype)
    hid = jax.nn.silu(jnp.einsum('td,edf->tef', xt, w_gate)) * jnp.einsum('td,edf->tef', xt, w_up)
    out = jnp.einsum('tef,efd->td', hid * comb[:, :, None], w_down)
    return out.reshape(B, L, D)


def setup_inputs(seed: int = 0) -> dict:
    key = jax.random.key(seed)
    ks = jax.random.split(key, 24)

    def nrm(k, shape, fan_in):
        return jax.random.normal(k, shape, jnp.float32) * (fan_in ** -0.5)

    def gain(k, shape):
        return 1.0 + 0.02 * jax.random.normal(k, shape, jnp.float32)

    def small(k, shape, s):
        return s * jax.random.normal(k, shape, jnp.float32)

    return {
        "x": jax.random.normal(ks[0], (BATCH, SEQ, D_MODEL), jnp.float32),
        "meta_tokens": jax.random.normal(ks[1], (N_META, D_MODEL), jnp.float32),
        "norm_mix_g": gain(ks[2], (DEPTH, D_MODEL)),
        "w_in": nrm(ks[3], (DEPTH, D_MODEL, D_IN), D_MODEL),
        "conv_dw_w": nrm(ks[4], (DEPTH, CONV_WIDTH, CONV_DIM), CONV_WIDTH),
        "conv_dw_b": small(ks[5], (DEPTH, CONV_DIM), 0.02),
        "conv_ln_g": gain(ks[6], (DEPTH, CONV_DIM)),
        "conv_ln_b": small(ks[7], (DEPTH, CONV_DIM), 0.02),
        "conv_pw_w": nrm(ks[8], (DEPTH, CONV_DIM, D_MODEL), CONV_DIM),
        "ret_gn_g": gain(ks[9], (DEPTH, RET_HEADS * RET_V_DIM)),
        "ret_w_o": nrm(ks[10], (DEPTH, RET_HEADS * RET_V_DIM, D_MODEL), RET_HEADS * RET_V_DIM),
        "w_out": nrm(ks[11], (DEPTH, D_MODEL, D_MODEL), D_MODEL),
        "norm_ffn_g": gain(ks[12], (DEPTH, D_MODEL)),
        "w_group_router": nrm(ks[13], (DEPTH, D_MODEL, N_GROUPS), D_MODEL),
        "b_group_router": small(ks[14], (DEPTH, N_GROUPS), 0.01),
        "w_expert_router": nrm(ks[15], (DEPTH, D_MODEL, N_EXPERTS), D_MODEL),
        "b_expert_router": small(ks[16], (DEPTH, N_EXPERTS), 0.01),
        "w_expert_gate": nrm(ks[17], (DEPTH, N_EXPERTS, D_MODEL, D_EXPERT), D_MODEL),
        "w_expert_up": nrm(ks[18], (DEPTH, N_EXPERTS, D_MODEL, D_EXPERT), D_MODEL),
        "w_expert_down": nrm(ks[19], (DEPTH, N_EXPERTS, D_EXPERT, D_MODEL), D_EXPERT),
        "norm_final_g": gain(ks[20], (D_MODEL,)),
    }


def reference(x, meta_tokens, norm_mix_g, w_in, conv_dw_w, conv_dw_b, conv_ln_g, conv_ln_b,
              conv_pw_w, ret_gn_g, ret_w_o, w_out, norm_ffn_g, w_group_router, b_group_router,
              w_expert_router, b_expert_router, w_expert_gate, w_expert_up, w_expert_down,
              norm_final_g):
    B = x.shape[0]
    meta = jnp.broadcast_to(meta_tokens[None].astype(x.dtype), (B, N_META, D_MODEL))
    h = jnp.concatenate([meta, x], axis=1)
    cuts = [int(c) for c in np.cumsum(IN_SPLITS)[:-1]]
    for layer in range(DEPTH):
        u = rmsnorm(h, norm_mix_g[layer])
        proj = u @ w_in[layer]
        conv_in, q, k, v, g_ret, g_merge = jnp.split(proj, cuts, axis=-1)
        y_conv = conv_module(conv_in, conv_dw_w[layer], conv_dw_b[layer], conv_ln_g[layer],
                             conv_ln_b[layer], conv_pw_w[layer])
        o_ret = retention(q, k, v, ret_gn_g[layer])
        y_ret = (jax.nn.silu(g_ret.astype(jnp.float32)) * o_ret).astype(h.dtype) @ ret_w_o[layer]
        gate_a, gate_b = jnp.split(g_merge, 2, axis=-1)
        merged = jax.nn.sigmoid(gate_a) * y_conv + jax.nn.sigmoid(gate_b) * y_ret
        h = h + merged @ w_out[layer]
        u = rmsnorm(h, norm_ffn_g[layer])
        h = h + hier_moe(u, w_group_router[layer], b_group_router[layer], w_expert_router[layer],
                         b_expert_router[layer], w_expert_gate[layer], w_expert_up[layer],
                         w_expert_down[layer])
    out = rmsnorm(h, norm_final_g)
    return out[:, N_META:]
```

```python
import numpy as np
from contextlib import ExitStack
import ml_dtypes
import concourse.bass as bass
import concourse.mybir as mybir
from concourse.bass_utils import run_bass_kernel_spmd

F32 = mybir.dt.float32
BF16 = mybir.dt.bfloat16
AF = mybir.ActivationFunctionType
ALU = mybir.AluOpType
AX = mybir.AxisListType

D = 1024
SEQ = 2048
NT = 16
NS = 4
NMETA = 16
D_IN = 10240
CW = 31
EPS = 1e-6
NE = 16
SC_OFF = (0, 128, 384, 768)
OFF_Q, OFF_K, OFF_V, OFF_G, OFF_GA, OFF_GB = 2048, 3072, 4096, 6144, 8192, 9216

ENGS = ("pe", "act", "dve", "pool", "sp")
UNIFIED_PSUM = True
HOIST = False
SYNC_WAW = True
PRE0 = True
MOE_PIPE = True
HOISTQK = True
HOISTG = True
PREQKA = False
HOISTV = True
PREDIAG = True
PREFA = False
PREQK = False
HOISTALL = False
HOISTGB = True
ROT_ENG = "dve"
CONV_IN_F = False
SYNC_ALL = True
DIAG_DVE = True


class Op:
    __slots__ = ("eng", "fn", "is_dma", "sem", "val", "seq", "inc", "cnt", "waits", "know")


class Tracker:
    def __init__(self, nc):
        self.nc = nc
        self.streams = {e: [] for e in ENGS}
        self.keys = {}
        self.eng_know = {e: {} for e in ENGS}
        self.dma_sems = {}
        self.comp_sem = {}
        self.nops = 0

    @staticmethod
    def _merge(a, b):
        for k, v in b.items():
            if a.get(k, -1) < v:
                a[k] = v

    def add(self, eng, fn, reads=(), writes=(), dma=None, extra=()):
        op = Op()
        op.eng, op.fn, op.is_dma, op.inc, op.cnt = eng, fn, dma is not None, False, 0
        op.seq = len(self.streams[eng])
        deps = []
        for k in reads:
            st = self.keys.get(k)
            if st is not None and st[0] is not None:
                deps.append((st[0], "raw"))
        for k in writes:
            st = self.keys.get(k)
            if st is not None:
                if st[0] is not None:
                    deps.append((st[0], "waw"))
                for r in st[1].values():
                    deps.append((r, "war"))
        for d in extra:
            deps.append((d, "raw"))
        if dma is not None:
            ds = self.dma_sems.get(dma)
            if ds is None:
                ds = [self.nc.alloc_semaphore("dsem_%d" % len(self.dma_sems)), 0, None]
                self.dma_sems[dma] = ds
            if ds[2] is not None:
                deps.append((ds[2], "raw"))
            ds[1] += 16
            op.sem, op.val = dma, ds[1]
            ds[2] = op
        else:
            op.sem, op.val = None, 0
        know = dict(self.eng_know[eng])
        waits = []
        deps.sort(key=lambda t: -(t[0].val if t[0].is_dma else t[0].seq))
        for d, kind in deps:
            if d is op:
                continue
            if d.is_dma:
                kk = ("d", d.sem)
                if know.get(kk, 0) >= d.val:
                    continue
                waits.append(d)
                self._merge(know, d.know)
                know[kk] = d.val
            else:
                if d.eng == eng and not op.is_dma:
                    if eng == "pe" or ((kind == "war" or (kind == "waw" and not SYNC_WAW)) and not SYNC_ALL):
                        continue
                if know.get(d.eng, -1) >= d.seq:
                    continue
                waits.append(d)
                d.inc = True
                self._merge(know, d.know)
                know[d.eng] = d.seq
        op.waits, op.know = waits, know
        self.eng_know[eng] = know
        self.streams[eng].append(op)
        for k in reads:
            st = self.keys.get(k)
            if st is None:
                st = [None, {}]
                self.keys[k] = st
            st[1][("d", op.sem) if op.is_dma else eng] = op
        for k in writes:
            self.keys[k] = [op, {}]
        self.nops += 1
        return op

    def lasts(self):
        out = []
        for e in ENGS:
            for op in reversed(self.streams[e]):
                if (not op.is_dma) and op.fn is not None:
                    out.append(op)
                    break
        for ds in self.dma_sems.values():
            if ds[2] is not None:
                out.append(ds[2])
        return out

    def barrier(self):
        ls = self.lasts()
        for e in ENGS:
            self.add(e, None, extra=ls)

    def emit(self, block):
        nc = self.nc
        for e in ("pe", "act", "dve", "pool"):
            self.comp_sem[e] = nc.alloc_semaphore("csem_" + e)
        for e in ENGS:
            c = 0
            for op in self.streams[e]:
                if op.inc:
                    c += 1
                op.cnt = c

        def run(ename, eng):
            for op in self.streams[ename]:
                for d in op.waits:
                    if d.is_dma:
                        eng.wait_ge(self.dma_sems[d.sem][0], d.val)
                    else:
                        eng.wait_ge(self.comp_sem[d.eng], d.cnt)
                if op.fn is None:
                    continue
                ins = op.fn(eng)
                if op.is_dma:
                    ins.then_inc(self.dma_sems[op.sem][0], 16)
                elif op.inc:
                    ins.then_inc(self.comp_sem[ename], 1)

        @block.tensor
        def _(eng):
            run("pe", eng)

        @block.scalar
        def _(eng):
            run("act", eng)

        @block.vector
        def _(eng):
            run("dve", eng)

        @block.gpsimd
        def _(eng):
            run("pool", eng)

        @block.sync
        def _(eng):
            run("sp", eng)


class RR:
    def __init__(self, name, bufs):
        self.name, self.bufs, self.i = name, bufs, 0

    def next(self):
        i = self.i
        self.i = (i + 1) % len(self.bufs)
        return self.bufs[i], (self.name, i)


def build_program(debug=False):
    nc = bass.Bass("TRN2", target_bir_lowering=False)
    T = Tracker(nc)

    def din(name, shape, dt=F32):
        return nc.dram_tensor(name, list(shape), dt, kind="ExternalInput").ap()

    x_d = din("x", [SEQ, D])
    meta_d = din("meta", [NMETA, D])
    w_in_d = din("w_in", [D, D_IN])
    w_pw_d = din("w_pw", [D, D])
    w_o_d = din("w_o", [2048, D])
    w_out_d = din("w_out", [D, D])
    w_rt_d = din("w_rt", [D, 20])
    w_g_d = din("w_gate", [NE, D, 512])
    w_u_d = din("w_up", [NE, D, 512])
    w_d_d = din("w_down", [NE, 512, D])
    ident_d = din("ident", [128, 128], BF16)
    cos_d = din("cos_t", [128, SEQ + NMETA])
    sin_d = din("sin_t", [128, SEQ + NMETA])
    mask_d = din("maskT", [128, 4, 4, 128])
    qdec_d = din("qdec", [128, 4, 128])
    kdec_d = din("kdec", [128, 4])
    kdecm_d = din("kdec_meta", [NMETA, 4])
    fmv_d = din("fmvec", [128, 6, 8])
    convw_d = din("convw_fm", [128, 8, CW])
    gn_d = din("gn_fm", [128, 16])
    brt_d = din("b_rt", [128, 20])
    gfin_d = din("gfin_b", [128, D])
    out_d = nc.dram_tensor("out", [SEQ, D], F32, kind="ExternalOutput").ap()
    dscr = nc.dram_tensor("diag_scr", [8, 128, CW * 128], BF16, kind="Internal").ap()

    def sb(name, shape, dt=F32):
        return nc.alloc_sbuf_tensor("s_" + name, list(shape), dt)

    dbg_ops = []

    def dbg(name, ap, shape, dt, keys):
        if not debug:
            return
        d = nc.dram_tensor("dbg_" + name, list(shape), dt, kind="ExternalOutput").ap()
        dbg_ops.append(T.add("sp", lambda e: e.dma_start(out=d, in_=ap), reads=keys, writes=[("dbg", name)], dma="dbg_" + name))

    def psum(name, shape, dt=F32):
        return nc.alloc_psum_tensor(name, list(shape), dt)

    h = sb("h", [128, NT, D])
    ident = sb("ident_sb", [128, 128], BF16)
    fmv = sb("fmv", [128, 6, 8])
    small = sb("small", [128, 64])
    small_i = [0]

    def sm(n=1):
        i = small_i[0]
        small_i[0] = (i + 4) % 64
        return small[:, i:i + n], ("small", i // 4)

    if UNIFIED_PSUM:
        NPS = 8
        psf = [psum("psf%d" % i, [128, 512]) for i in range(NPS)]
    else:
        NPS = 6
        psf = [psum("psf%d" % i, [128, 512]) for i in range(NPS)]
        pst = [psum("pst%d" % i, [128, 1024], BF16) for i in range(2)]
    ps_i = [0]
    ps_pinned = set()

    def ps_next():
        while True:
            i = ps_i[0]
            ps_i[0] = (i + 1) % NPS
            if i not in ps_pinned:
                return psf[i], ("psf", i)

    class _PstRR:
        def next(self):
            bank, key = ps_next()
            return bank[:, :].bitcast(BF16), key

    pst_rr = _PstRR() if UNIFIED_PSUM else RR("pst", pst)

    def dma(queue, out, in_, reads, writes, sem):
        return T.add(queue, lambda e: e.dma_start(out=out, in_=in_), reads=reads, writes=writes, dma=sem)

    dma("sp", ident[:], ident_d[:, :], [], ["ident"], "c0")
    dma("sp", fmv[:], fmv_d[:, :, :], [], ["fmv"], "c1")

    GMIX, GFFN, CONVB, LNG, LNB = 0, 1, 2, 3, 4

    mst = ExitStack()
    cur = {}

    def msb(name, shape, dt=F32):
        return mst.enter_context(nc.sbuf_tensor("m_" + name, list(shape), dt))

    wslots = [msb("wslot%d" % i, [128, 4096], BF16) for i in range(3)]
    w_rr = RR("wslot", wslots)
    uT = msb("uT", [128, 8, 512], BF16)
    uTm = msb("uTm", [128, 8, NMETA], BF16)
    hc = msb("hc", [128, 8, 542], BF16)
    cs = msb("cs", [128, 8, 512], BF16)
    ycg = msb("ycg", [128, 8, 512], BF16)
    qk = msb("qk", [128, 4, 512], BF16)
    qp = msb("qp", [128, 2, 512], BF16)
    kd = msb("kd", [128, 4, 256], BF16)
    vv = msb("vv", [128, 4, 512], BF16)
    gg = msb("gg", [128, 4, 512], BF16)
    oT = msb("oT", [128, 16, 512], BF16)
    S = msb("S", [128, 8, 512])
    Sbf = msb("Sbf", [128, 2, 512], BF16)
    cos_sb = msb("cos_sb", [128, 512])
    sin_sb = msb("sin_sb", [128, 512])
    cosm = msb("cosm", [128, NMETA])
    sinm = msb("sinm", [128, NMETA])
    maskT = msb("maskT", [128, 4, 4, 128], BF16)
    qdec = msb("qdec", [128, 4, 128])
    kdec = msb("kdec", [128, 4])
    kdecm = msb("kdecm", [NMETA, 4])
    convw = msb("convw", [128, 8, CW])
    gn_fm = msb("gn_fm", [128, 16])
    ones_bf = msb("ones_bf", [128, 128], BF16)
    tf_rr = RR("tf", [msb("tf%d" % i, [128, 512]) for i in range(3)])
    sq_rr = RR("sq", [msb("sq%d" % i, [128, 512], BF16) for i in range(2)])
    cur["u_rr"] = RR("u_tm", [msb("u_tm%d" % i, [128, D], BF16) for i in range(2)])
    sc_all = msb("sc_all", [128, 1280], BF16)
    og_rr = RR("og", [msb("og%d" % i, [128, 512], BF16) for i in range(4)])
    kmeta = msb("kmeta", [128, 2, NMETA], BF16)
    kdm = msb("kdm", [NMETA, 256], BF16)
    vm = msb("vm", [NMETA, 512], BF16)
    st6 = msb("st6", [128, 4, 6])
    stat2 = msb("stat2", [128, 2, 512])
    mu_b = stat2[:, 0, :]
    rs_b = stat2[:, 1, :]
    xm = stat2[0:NMETA, :, :].rearrange("p a b -> p (a b)")
    mv = msb("mv", [128, 4, 2])

    dma("pool", maskT[:], mask_d[:, :, :, :], [], ["maskT"], "m0")
    dma("sp", qdec[:], qdec_d[:, :, :], [], ["qdec"], "c3")
    dma("sp", kdec[:], kdec_d[:, :], [], ["kdec"], "c0")
    dma("sp", kdecm[:], kdecm_d[:, :], [], ["kdecm"], "c1")
    dma("sp", convw[:], convw_d[:, :, :], [], ["convw"], "c2")
    dma("sp", gn_fm[:], gn_d[:, :], [], ["gn_fm"], "c3")
    dma("sp", cosm[:], cos_d[:, 0:NMETA], [], ["cosm"], "c0")
    dma("sp", sinm[:], sin_d[:, 0:NMETA], [], ["sinm"], "c1")
    T.add("dve", lambda e: e.memset(ones_bf[:], 1.0), writes=["ones"])
    T.add("dve", lambda e: e.memset(hc[:, :, 0:14], 0.0), writes=["hc_hist"])

    gam = [1.0 - 2.0 ** (-5.0 - i) for i in range(4)]
    lgam = [float(np.log(np.float32(1.0) - np.float32(2.0) ** np.float32(-5.0 - i))) for i in range(4)]

    def gpow(hd, e):
        return float(np.exp(lgam[hd] * e))

    g512 = [gpow(hd, 512) for hd in range(4)]

    def wload(parts):
        slot, key = w_rr.next()
        for k, (dstf, src) in enumerate(parts):
            dma("pool", dstf(slot), src, [], [key], "w%d" % key[1])
        return slot, key

    w_in_v = w_in_d.rearrange("(kc p) n -> p kc n", p=128)
    w_pw_v = w_pw_d.rearrange("(kc p) n -> p kc n", p=128)
    w_o_v = w_o_d.rearrange("(kc p) n -> p kc n", p=128)
    w_out_v = w_out_d.rearrange("(kc p) n -> p kc n", p=128)

    def v8(slot):
        return slot[:].rearrange("p (k c) -> p k c", c=512)

    def v16(slot):
        return slot[:].rearrange("p (k c) -> p k c", c=256)

    def load_cols(view, c0, n=512):
        return wload([(lambda s: v8(s)[:, :, 0:n], view[:, :, c0:c0 + n])])

    def load_cols2(view, ca, cb):
        return wload([(lambda s: v8(s)[:, :, 0:256], view[:, :, ca:ca + 256]),
                      (lambda s: v8(s)[:, :, 256:512], view[:, :, cb:cb + 256])])

    def rms_to_fm(src_ap, npart, dst_fn, src_key, dst_key, gsel):
        u_tm, uk = cur["u_rr"].next()
        skeys = src_key if isinstance(src_key, list) else [src_key]
        ssA, ssK = sm()
        T.add("act", lambda e: e.activation(out=u_tm[0:npart, :], in_=src_ap, func=AF.Square, accum_out=ssA[0:npart, :]),
              reads=skeys, writes=[ssK, uk])
        rsA, rsK = sm()
        T.add("dve", lambda e: e.tensor_scalar(out=rsA[0:npart, :], in0=ssA[0:npart, :], scalar1=1.0 / D, scalar2=EPS,
                                               op0=ALU.mult, op1=ALU.add), reads=[ssK], writes=[rsK])
        sdA, sdK = sm()
        T.add("act", lambda e: e.activation(out=sdA[0:npart, :], in_=rsA[0:npart, :], func=AF.Sqrt), reads=[rsK], writes=[sdK])
        rA, rK = sm()
        T.add("dve", lambda e: e.reciprocal(out=rA[0:npart, :], in_=sdA[0:npart, :]), reads=[sdK], writes=[rK])
        T.add("dve", lambda e: e.tensor_scalar(out=u_tm[0:npart, :], in0=src_ap, scalar1=rA[0:npart, :], scalar2=None,
                                               op0=ALU.mult), reads=skeys + [rK], writes=[uk])
        pt, ptk = pst_rr.next()

        def tr(e):
            ins = None
            for k in range(8):
                ins = e.transpose(pt[:, k * npart:(k + 1) * npart], u_tm[0:npart, k * 128:(k + 1) * 128],
                                  ident[0:npart, 0:npart])
            return ins
        T.add("pe", tr, reads=[uk, "ident"], writes=[ptk])
        T.add("dve", lambda e: e.tensor_tensor(
            out=dst_fn(), in0=pt[:, 0:8 * npart].rearrange("p (k n) -> p k n", n=npart),
            in1=fmv[:, gsel, :].unsqueeze(2).to_broadcast([128, 8, npart]), op=ALU.mult),
            reads=[ptk, "fmv"], writes=[dst_key])

    def rms_part1(items, npre=0):
        n = len(items)
        ssA, ssK = sm(4)
        for t, (src_ap, src_key, dst_fn, dst_key) in enumerate(items):
            junk, jk = cur["u_rr"].next()
            T.add("act", lambda e, junk=junk, src_ap=src_ap, t=t: e.activation(out=junk[:], in_=src_ap, func=AF.Square,
                                                                              accum_out=ssA[:, t:t + 1]),
                  reads=[src_key], writes=[(ssK, t), jk])
        ss_keys = [(ssK, t) for t in range(n)]
        rsA, rsK = sm(4)
        T.add("dve", lambda e: e.tensor_scalar(out=rsA[:, 0:n], in0=ssA[:, 0:n], scalar1=1.0 / D, scalar2=EPS,
                                               op0=ALU.mult, op1=ALU.add), reads=ss_keys, writes=[rsK])
        sdA, sdK = sm(4)
        T.add("act", lambda e: e.activation(out=sdA[:, 0:n], in_=rsA[:, 0:n], func=AF.Sqrt), reads=[rsK], writes=[sdK])
        rA, rK = sm(4)
        T.add("dve", lambda e: e.reciprocal(out=rA[:, 0:n], in_=sdA[:, 0:n]), reads=[sdK], writes=[rK])
        pre = []
        for t in range(npre):
            pre.append(rms_scale(items[t], t, rA, rK))
        return dict(items=items, rA=rA, rK=rK, pre=pre)

    def rms_scale(item, t, rA, rK):
        src_ap, src_key, dst_fn, dst_key = item
        u_tm, uk = cur["u_rr"].next()
        T.add("act", lambda e: e.activation(out=u_tm[:], in_=src_ap, func=AF.Copy, scale=rA[:, t:t + 1]),
              reads=[src_key, rK], writes=[uk])
        return u_tm, uk

    def rms_part2(ctx, gsel):
        items, rA, rK = ctx["items"], ctx["rA"], ctx["rK"]
        for t, item in enumerate(items):
            src_ap, src_key, dst_fn, dst_key = item
            u_tm, uk = ctx["pre"][t] if t < len(ctx["pre"]) else rms_scale(item, t, rA, rK)
            pt, ptk = pst_rr.next()

            def tr(e, pt=pt, u_tm=u_tm):
                ins = None
                for k in range(8):
                    ins = e.transpose(pt[:, k * 128:(k + 1) * 128], u_tm[:, k * 128:(k + 1) * 128], ident[:])
                return ins
            T.add("pe", tr, reads=[uk, "ident"], writes=[ptk])
            T.add("dve", lambda e, pt=pt, dst_fn=dst_fn: e.tensor_tensor(
                out=dst_fn(), in0=pt[:, :].rearrange("p (k n) -> p k n", n=128),
                in1=fmv[:, gsel, :].unsqueeze(2).to_broadcast([128, 8, 128]), op=ALU.mult),
                reads=[ptk, "fmv"], writes=[dst_key])

    def rms_batch(items, gsel):
        rms_part2(rms_part1(items), gsel)

    def mm_group(out_ap, pairs, reads, wkey):
        n = len(pairs)

        def fn(e):
            ins = None
            for i, (l, r) in enumerate(pairs):
                ins = e.matmul(out_ap, lhsT=l, rhs=r, start=(i == 0), stop=(i == n - 1))
            return ins
        return T.add("pe", fn, reads=reads, writes=[wkey])

    prefA_ctx = [None]
    prediag = {}
    for s in range(NS):
        c0 = NMETA + 512 * s
        tiles = range(4 * s, 4 * s + 4)
        uT_keys = [("uT", t) for t in range(4)]
        def a_items(ss):
            return [(h[:, 4 * ss + t, :], ("h", 4 * ss + t), (lambda t=t: uT[:, :, 128 * t:128 * (t + 1)]), ("uT", t)) for t in range(4)]

        def load_x(ss):
            for i in range(4 * ss, 4 * ss + 4):
                dma("sp", h[:, i, :], x_d[128 * i:128 * (i + 1), :], [], [("h", i)], "x%d" % (i % 4))

        def load_rot(ss):
            cc = NMETA + 512 * ss
            dma("sp", cos_sb[:], cos_d[:, cc:cc + 512], [], ["cos"], "c2")
            dma("sp", sin_sb[:], sin_d[:, cc:cc + 512], [], ["sin"], "c3")

        if s == 0 or not PREFA:
            load_rot(s)
            if s == 0:
                dma("sp", xm, meta_d[:, :], [], ["mu_b", "rs_b"], "xm")
                rms_to_fm(xm, NMETA, lambda: uTm[:, :, :], ["mu_b", "rs_b"], "uTm", GMIX)
            load_x(s)
            rms_batch(a_items(s), GMIX)
        if PREFA and s + 1 < NS:
            load_x(s + 1)

        if s == 0:
            dbg("uT0", uT[:], [128, 8, 512], BF16, uT_keys)
            dbg("uTm", uTm[:], [128, 8, NMETA], BF16, ["uTm"])
        if s > 0:
            T.add("dve", lambda e: e.tensor_copy(out=hc[:, :, 0:30], in_=hc[:, :, 512:542]),
                  reads=[("hc", j) for j in range(8)], writes=["hc_hist"])
        for cg in range(4):
            slot, wk = load_cols2(w_in_v, 256 * cg, 1024 + 256 * cg)
            sv = v8(slot)
            for jj in range(2):
                j = 2 * cg + jj
                pa, pak = ps_next()
                mm_group(pa[:, :], [(sv[:, k, jj * 128:(jj + 1) * 128], uT[:, k, :]) for k in range(8)],
                         [wk] + uT_keys, pak)
                pg, pgk = ps_next()
                mm_group(pg[:, :], [(sv[:, k, 256 + jj * 128:256 + (jj + 1) * 128], uT[:, k, :]) for k in range(8)],
                         [wk] + uT_keys, pgk)
                tf, tfk = tf_rr.next()
                T.add("act", lambda e, pg=pg, tf=tf: e.activation(out=tf[:], in_=pg[:, :], func=AF.Sigmoid),
                      reads=[pgk], writes=[tfk])
                T.add("dve", lambda e, pa=pa, tf=tf, j=j: e.tensor_tensor(out=hc[:, j, 30:542], in0=pa[:, :], in1=tf[:], op=ALU.mult),
                      reads=[pak, tfk], writes=[("hc", j)])
                if s == 0:
                    pa2, pak2 = ps_next()
                    mm_group(pa2[:, 0:NMETA], [(sv[:, k, jj * 128:(jj + 1) * 128], uTm[:, k, :]) for k in range(8)],
                             [wk, "uTm"], pak2)
                    mm_group(pa2[:, 32:32 + NMETA], [(sv[:, k, 256 + jj * 128:256 + (jj + 1) * 128], uTm[:, k, :]) for k in range(8)],
                             [wk, "uTm"], pak2)
                    tf2, tfk2 = tf_rr.next()
                    T.add("act", lambda e, p=pa2, tf=tf2: e.activation(out=tf[:, 0:NMETA], in_=p[:, 32:32 + NMETA], func=AF.Sigmoid),
                          reads=[pak2], writes=[tfk2])
                    T.add("dve", lambda e, p=pa2, tf=tf2, j=j: e.tensor_tensor(out=hc[:, j, 14:30], in0=p[:, 0:NMETA], in1=tf[:, 0:NMETA], op=ALU.mult),
                          reads=[pak2, tfk2], writes=[("hcm", j)])

        if s == 0:
            dbg("hc0", hc[:], [128, 8, 542], BF16, [("hc", j) for j in range(8)] + [("hcm", j) for j in range(8)] + ["hc_hist"])
        if s == 0 and PREDIAG:
            for j in range(6):
                hv = h[:, 4 + 2 * j:6 + 2 * j, :].rearrange("p a b -> p (a b)").bitcast(BF16)
                dv3 = hv[:, 0:CW * 128].rearrange("p (t c) -> p t c", c=128)
                hk = [("h", 4 + 2 * j), ("h", 5 + 2 * j)]
                seen = set()
                for tap in range(CW):
                    who = ("dve", "act", "pool", "act", "dve", "act", "dve", "act")[tap % 8]
                    first = who not in seen
                    seen.add(who)
                    wr = (hk if first else []) + [("pd", j, who)]
                    if who == "act":
                        T.add("act", lambda e, dv3=dv3, j=j, tap=tap: e.activation(out=dv3[:, tap, :], in_=ident[:], func=AF.Copy,
                                                                                  scale=convw[:, j, tap:tap + 1]),
                              reads=["ident", "convw"], writes=wr)
                    elif who == "dve":
                        T.add("dve", lambda e, dv3=dv3, j=j, tap=tap: e.tensor_scalar(out=dv3[:, tap, :], in0=ident[:],
                                                                                     scalar1=convw[:, j, tap:tap + 1], scalar2=None,
                                                                                     op0=ALU.mult),
                              reads=["ident", "convw"], writes=wr)
                    else:
                        T.add("pool", lambda e, dv3=dv3, j=j, tap=tap: e.tensor_scalar(out=dv3[:, tap, :], in0=ident[:],
                                                                                      scalar1=convw[:, j, tap:tap + 1], scalar2=1.0,
                                                                                      op0=ALU.mult, op1=ALU.mult),
                              reads=["ident", "convw"], writes=wr)
                prediag[j] = (hv, dv3, hk + [("pd", j, w) for w in ("act", "dve", "pool")])
                dma("sp", dscr[j], hv[:, 0:CW * 128], prediag[j][2], [("dscr", j)], "dscr%d" % (j % 2))
        s1, s1k = ps_next()
        ps_pinned.add(s1k[1])
        s2, s2k = ps_next()
        ps_pinned.add(s2k[1])
        def conv_chunk_slot(j, pc, pck):
            slot, wk = w_rr.next()
            dv3 = slot[:, 0:CW * 128].rearrange("p (t c) -> p t c", c=128)
            if s == 0:
                seen = set()
                for tap in range(CW):
                    who = (("dve", "act", "dve", "act", "dve", "pool", "dve", "act") if DIAG_DVE else ("act", "pool", "act", "act", "act", "pool", "act", "act"))[tap % 8]
                    first = who not in seen
                    seen.add(who)
                    wr = ([wk] if first else []) + [(wk, who)]
                    if who == "act":
                        T.add("act", lambda e, dv3=dv3, j=j, tap=tap: e.activation(out=dv3[:, tap, :], in_=ident[:], func=AF.Copy,
                                                                                  scale=convw[:, j, tap:tap + 1]),
                              reads=["ident", "convw"], writes=wr)
                    elif who == "dve":
                        T.add("dve", lambda e, dv3=dv3, j=j, tap=tap: e.tensor_scalar(out=dv3[:, tap, :], in0=ident[:],
                                                                                     scalar1=convw[:, j, tap:tap + 1], scalar2=None,
                                                                                     op0=ALU.mult),
                              reads=["ident", "convw"], writes=wr)
                    else:
                        T.add("pool", lambda e, dv3=dv3, j=j, tap=tap: e.tensor_scalar(out=dv3[:, tap, :], in0=ident[:],
                                                                                      scalar1=convw[:, j, tap:tap + 1], scalar2=1.0,
                                                                                      op0=ALU.mult, op1=ALU.mult),
                              reads=["ident", "convw"], writes=wr)
                    T.add("pe", lambda e, pc=pc, dv3=dv3, j=j, tap=tap: e.matmul(pc[:, :], lhsT=dv3[:, tap, :], rhs=hc[:, j, tap:tap + 512],
                                                                                start=(tap == 0), stop=(tap == CW - 1)),
                          reads=[wk, (wk, who), ("hc", j), ("hcm", j), "hc_hist"], writes=[pck])
                dma("sp", dscr[j], slot[:, 0:CW * 128], [wk, (wk, "act"), (wk, "dve"), (wk, "pool")], [("dscr", j)], "dscr%d" % (j % 2))
            else:
                dma("sp", slot[:, 0:CW * 128], dscr[j], [("dscr", j)], [wk], "wdg%d" % wk[1])

                def taps(e, pc=pc, dv3=dv3, j=j):
                    ins = None
                    for tap in range(CW):
                        ins = e.matmul(pc[:, :], lhsT=dv3[:, tap, :], rhs=hc[:, j, tap:tap + 512], start=(tap == 0), stop=(tap == CW - 1))
                    return ins
                T.add("pe", taps, reads=[wk, ("hc", j), ("hcm", j), "hc_hist"], writes=[pck])

        def conv_chunk(j):
            pc, pck = ps_next()
            if s == 0 and j in prediag:
                hv, dv3, pkeys = prediag[j]

                def taps0(e, pc=pc, dv3=dv3, j=j):
                    ins = None
                    for tap in range(CW):
                        ins = e.matmul(pc[:, :], lhsT=dv3[:, tap, :], rhs=hc[:, j, tap:tap + 512], start=(tap == 0), stop=(tap == CW - 1))
                    return ins
                T.add("pe", taps0, reads=pkeys + [("hc", j), ("hcm", j), "hc_hist"], writes=[pck])
            else:
                conv_chunk_slot(j, pc, pck)
            T.add("act", lambda e, pc=pc, j=j: e.activation(out=cs[:, j, :], in_=pc[:, :], func=AF.Identity,
                                                            bias=fmv[:, CONVB, j:j + 1]),
                  reads=[pck, "fmv"], writes=[("cs", j)])
            sq, sqk = sq_rr.next()
            T.add("dve", lambda e, sq=sq, j=j: e.tensor_tensor(out=sq[:], in0=cs[:, j, :], in1=cs[:, j, :], op=ALU.mult),
                  reads=[("cs", j)], writes=[sqk])
            T.add("pe", lambda e, j=j, s1=s1: e.matmul(s1[:, :], lhsT=ones_bf[:], rhs=cs[:, j, :], start=(j == 0), stop=(j == 7)),
                  reads=["ones", ("cs", j)], writes=[s1k])
            T.add("pe", lambda e, j=j, sq=sq, s2=s2: e.matmul(s2[:, :], lhsT=ones_bf[:], rhs=sq[:], start=(j == 0), stop=(j == 7)),
                  reads=["ones", sqk], writes=[s2k])

        if not CONV_IN_F:
            for j in range(8):
                conv_chunk(j)
        if s == 0:
            dbg("cspre0", cs[:], [128, 8, 512], BF16, [("cs", j) for j in range(8)])
        def emit_gate_b():
            for gi in range(2):
                slot, wk = load_cols(w_in_v, OFF_GB + 512 * gi)
                sv = v8(slot)
                for dd in range(4):
                    d = 4 * gi + dd
                    p, pk = ps_next()
                    mm_group(p[:, :], [(sv[:, k, dd * 128:(dd + 1) * 128], uT[:, k, :]) for k in range(8)], [wk] + uT_keys, pk)
                    T.add("act", lambda e, p=p, d=d: e.activation(out=cs[:, d, :], in_=p[:, :], func=AF.Sigmoid),
                          reads=[pk], writes=[("cs", d)])

        def emit_v(hd):
                slot, wk = load_cols(w_in_v, OFF_V + 512 * hd)
                sv = v8(slot)
                for t in range(4):
                    p, pk = ps_next()
                    mm_group(p[:, :], [(uT[:, k, 128 * t:128 * (t + 1)], sv[:, k, :]) for k in range(8)], [wk, ("uT", t)], pk)
                    T.add("act", lambda e, p=p, t=t: e.activation(out=vv[:, t, :], in_=p[:, :], func=AF.Copy), reads=[pk], writes=[("vv", t)])
                if s == 0:
                    p, pk = ps_next()
                    mm_group(p[0:NMETA, :], [(uTm[:, k, :], sv[:, k, :]) for k in range(8)], [wk, "uTm"], pk)
                    T.add("act", lambda e, p=p: e.activation(out=vm[:], in_=p[0:NMETA, :], func=AF.Copy), reads=[pk], writes=["vm"])

        def emit_sinit(hd):
            for c in range(2):
                p2, p2k = ps_next()
                mm_group(p2[:, :], [(kdm[:, c * 128:(c + 1) * 128], vm[:])], ["kdm", "vm"], p2k)
                T.add("dve", lambda e, p2=p2, c=c, hd=hd: e.tensor_copy(out=S[:, 2 * hd + c, :], in_=p2[:, :]),
                      reads=[p2k], writes=[("S", hd)])

        def emit_g(hd):
                slot, wk = load_cols(w_in_v, OFF_G + 512 * hd)
                sv = v8(slot)
                for t in range(4):
                    p, pk = ps_next()
                    mm_group(p[:, :], [(uT[:, k, 128 * t:128 * (t + 1)], sv[:, k, :]) for k in range(8)], [wk, ("uT", t)], pk)
                    T.add("act", lambda e, p=p, t=t: e.activation(out=gg[:, t, :], in_=p[:, :], func=AF.Silu), reads=[pk], writes=[("gg", t)])

        def emit_qkA(hd):
            def rotary(pa, pak, pb, pbk, o1, o2, ncol, ct, st, ckeys, okey):
                t1, t1k = tf_rr.next()
                t2, t2k = tf_rr.next()
                T.add("dve", lambda e: e.tensor_tensor(out=t1[:, 0:ncol], in0=pa, in1=ct, op=ALU.mult), reads=[pak] + ckeys, writes=[t1k])
                T.add("dve", lambda e: e.tensor_tensor(out=t2[:, 0:ncol], in0=pb, in1=st, op=ALU.mult), reads=[pbk] + ckeys, writes=[t2k])
                T.add(ROT_ENG, lambda e: e.tensor_tensor(out=o1, in0=t1[:, 0:ncol], in1=t2[:, 0:ncol], op=ALU.subtract),
                      reads=[t1k, t2k], writes=[okey])
                t3, t3k = tf_rr.next()
                t4, t4k = tf_rr.next()
                T.add("dve", lambda e: e.tensor_tensor(out=t3[:, 0:ncol], in0=pb, in1=ct, op=ALU.mult), reads=[pbk] + ckeys, writes=[t3k])
                T.add("dve", lambda e: e.tensor_tensor(out=t4[:, 0:ncol], in0=pa, in1=st, op=ALU.mult), reads=[pak] + ckeys, writes=[t4k])
                T.add(ROT_ENG, lambda e: e.tensor_tensor(out=o2, in0=t3[:, 0:ncol], in1=t4[:, 0:ncol], op=ALU.add),
                      reads=[t3k, t4k], writes=[okey])

            slot, wk = load_cols2(w_in_v, OFF_Q + 256 * hd, OFF_K + 256 * hd)
            sv = v8(slot)
            banks = []
            for c in range(2):
                p, pk = ps_next()
                mm_group(p[:, :], [(sv[:, k, c * 128:(c + 1) * 128], uT[:, k, :]) for k in range(8)], [wk] + uT_keys, pk)
                banks.append((p, pk))
            rotary(banks[0][0][:, :], banks[0][1], banks[1][0][:, :], banks[1][1], qk[:, 0, :], qk[:, 1, :], 512,
                   cos_sb[:], sin_sb[:], ["cos", "sin"], "qk_q")
            for c in range(2, 4):
                p, pk = ps_next()
                mm_group(p[:, :], [(sv[:, k, c * 128:(c + 1) * 128], uT[:, k, :]) for k in range(8)], [wk] + uT_keys, pk)
                banks.append((p, pk))
            if s == 0:
                pm, pmk = ps_next()
                for c in range(2):
                    mm_group(pm[:, 32 * c:32 * c + NMETA],
                             [(sv[:, k, (2 + c) * 128:(3 + c) * 128], uTm[:, k, :]) for k in range(8)], [wk, "uTm"], pmk)
            rotary(banks[2][0][:, :], banks[2][1], banks[3][0][:, :], banks[3][1], qk[:, 2, :], qk[:, 3, :], 512,
                   cos_sb[:], sin_sb[:], ["cos", "sin"], "qk_k")
            if s == 0:
                rotary(pm[:, 0:NMETA], pmk, pm[:, 32:32 + NMETA], pmk, kmeta[:, 0, :], kmeta[:, 1, :], NMETA,
                       cosm[:], sinm[:], ["cosm", "sinm"], "kmeta")
                pt, ptk = pst_rr.next()

                def trm(e, pt=pt):
                    ins = None
                    for c in range(2):
                        ins = e.transpose(pt[0:NMETA, c * 128:(c + 1) * 128], kmeta[:, c, :], ident[:])
                    return ins
                T.add("pe", trm, reads=["kmeta", "ident"], writes=[ptk])
                T.add("dve", lambda e, pt=pt, hd=hd: e.tensor_scalar(out=kdm[:], in0=pt[0:NMETA, 0:256], scalar1=kdecm[:, hd:hd + 1],
                                                                     scalar2=None, op0=ALU.mult),
                      reads=[ptk, "kdecm"], writes=["kdm"])

        def emit_qkB(hd):
            for t in range(4):
                T.add("dve", lambda e, hd=hd, t=t: e.scalar_tensor_tensor(
                    out=qp[:, :, 128 * t:128 * (t + 1)], in0=qk[:, 0:2, 128 * t:128 * (t + 1)], scalar=gpow(hd, 128 * t),
                    in1=qdec[:, hd, :].unsqueeze(1).to_broadcast([128, 2, 128]), op0=ALU.mult, op1=ALU.mult),
                    reads=["qk_q", "qdec"], writes=["qp"])
            for t in range(4):
                pt, ptk = pst_rr.next()

                def trk(e, pt=pt, t=t):
                    ins = None
                    for c in range(2):
                        ins = e.transpose(pt[:, c * 128:(c + 1) * 128], qk[:, 2 + c, 128 * t:128 * (t + 1)], ident[:])
                    return ins
                T.add("pe", trk, reads=["qk_k", "ident"], writes=[ptk])
                T.add("dve", lambda e, pt=pt, t=t, hd=hd: e.tensor_scalar(out=kd[:, t, :], in0=pt[:, 0:256], scalar1=kdec[:, hd:hd + 1],
                                                                          scalar2=gpow(hd, 384 - 128 * t), op0=ALU.mult, op1=ALU.mult),
                      reads=[ptk, "kdec"], writes=[("kd", t)])


        def emit_qk(hd):
            emit_qkA(hd)
            emit_qkB(hd)

        if PRE0 and not CONV_IN_F:
            if PREQK:
                emit_qk(0)
            emit_v(0)
            emit_g(0)
        def stage_DE():
            mu, muk = mu_b, "mu_b"
            T.add("act", lambda e, mu=mu, s1=s1: e.activation(out=mu, in_=s1[:, :], func=AF.Copy, scale=1.0 / D), reads=[s1k], writes=[muk])
            msq, msqk = rs_b, "rs_b"
            T.add("dve", lambda e, mu=mu, msq=msq: e.tensor_tensor(out=msq, in0=mu, in1=mu, op=ALU.mult), reads=[muk], writes=[msqk])
            T.add("dve", lambda e, msq=msq, s2=s2: e.scalar_tensor_tensor(out=msq, in0=s2[:, :], scalar=1.0 / D, in1=msq,
                                                          op0=ALU.mult, op1=ALU.subtract), reads=[s2k, msqk], writes=[msqk])
            T.add("dve", lambda e, msq=msq: e.tensor_scalar(out=msq, in0=msq, scalar1=EPS, scalar2=None, op0=ALU.add),
                  reads=[msqk], writes=[msqk])
            T.add("act", lambda e, msq=msq: e.activation(out=msq, in_=msq, func=AF.Sqrt), reads=[msqk], writes=[msqk])
            T.add("dve", lambda e, msq=msq: e.reciprocal(out=msq, in_=msq), reads=[msqk], writes=[msqk])
            ps_pinned.discard(s1k[1])
            ps_pinned.discard(s2k[1])
            for j in range(8):
                tf, tfk = tf_rr.next()
                T.add("dve", lambda e, tf=tf, j=j, mu=mu: e.tensor_tensor(out=tf[:], in0=cs[:, j, :], in1=mu, op=ALU.subtract),
                      reads=[("cs", j), muk], writes=[tfk])
                T.add("dve", lambda e, tf=tf, msq=msq: e.tensor_tensor(out=tf[:], in0=tf[:], in1=msq, op=ALU.mult),
                      reads=[tfk, msqk], writes=[tfk])
                T.add("act", lambda e, tf=tf, j=j: e.activation(out=cs[:, j, :], in_=tf[:], func=AF.Silu,
                                                                scale=fmv[:, LNG, j:j + 1], bias=fmv[:, LNB, j:j + 1]),
                      reads=[tfk, "fmv"], writes=[("cs", j)])
            cs_keys = [("cs", j) for j in range(8)]
            if s == 0:
                dbg("cs0", cs[:], [128, 8, 512], BF16, cs_keys)
            for gi in range(2):
                slot, wk = load_cols(w_in_v, OFF_GA + 512 * gi)
                sv = v8(slot)
                for dd in range(4):
                    d = 4 * gi + dd
                    p, pk = ps_next()
                    mm_group(p[:, :], [(sv[:, k, dd * 128:(dd + 1) * 128], uT[:, k, :]) for k in range(8)], [wk] + uT_keys, pk)
                    T.add("act", lambda e, p=p, d=d: e.activation(out=ycg[:, d, :], in_=p[:, :], func=AF.Sigmoid),
                          reads=[pk], writes=[("ycg", d)])
            if PREQKA and not CONV_IN_F:
                emit_qkA(0)
            for gi in range(2):
                slot, wk = load_cols(w_pw_v, 512 * gi)
                sv = v8(slot)
                for dd in range(4):
                    d = 4 * gi + dd
                    p, pk = ps_next()
                    mm_group(p[:, :], [(sv[:, k, dd * 128:(dd + 1) * 128], cs[:, k, :]) for k in range(8)], [wk] + cs_keys, pk)
                    T.add("dve", lambda e, p=p, d=d: e.tensor_tensor(out=ycg[:, d, :], in0=p[:, :], in1=ycg[:, d, :], op=ALU.mult),
                          reads=[pk, ("ycg", d)], writes=[("ycg", d)])

            if s == 0:
                dbg("ycgE0", ycg[:], [128, 8, 512], BF16, [("ycg", d) for d in range(8)])
        if not CONV_IN_F:
            stage_DE()
        for hd in range(4):
            if hd == 0 and PREQKA and not PREQK:
                emit_qkB(0)
            elif (hd == 0 and not PREQK) or (hd > 0 and not HOISTQK):
                emit_qk(hd)
            if (hd == 0 and not (PRE0 and not CONV_IN_F)) or (hd > 0 and not HOIST and not HOISTALL and not (HOISTQK and HOISTV)):
                emit_v(hd)
            if s == 0 and (hd == 0 or not HOISTALL):
                emit_sinit(hd)
            if not (PRE0 and not CONV_IN_F and hd == 0) and not (HOISTALL and hd > 0) and not (HOISTG and HOISTQK and HOISTV and hd > 0):
                emit_g(hd)
            if s == 0 and hd == 0:
                dbg("qk00", qk[:], [128, 4, 512], BF16, ["qk_q", "qk_k"])
                dbg("qp00", qp[:], [128, 2, 512], BF16, ["qp"])
                dbg("kd00", kd[:], [128, 4, 256], BF16, [("kd", t) for t in range(4)])
                dbg("vv00", vv[:], [128, 4, 512], BF16, [("vv", t) for t in range(4)])
                dbg("gg00", gg[:], [128, 4, 512], BF16, [("gg", t) for t in range(4)])
                dbg("S00", S[:, 0:2, :], [128, 2, 512], F32, [("S", 0)])
            T.add("act", lambda e, hd=hd: e.activation(out=Sbf[:], in_=S[:, 2 * hd:2 * hd + 2, :], func=AF.Copy),
                  reads=[("S", hd)], writes=["Sbf"])
            if CONV_IN_F:
                pkv = []
                for c in range(2):
                    p2, p2k = ps_next()
                    mm_group(p2[:, :], [(kd[:, t, c * 128:(c + 1) * 128], vv[:, t, :]) for t in range(4)],
                             [("kd", t) for t in range(4)] + [("vv", t) for t in range(4)], p2k)
                    pkv.append((p2, p2k))
                for c in range(2):
                    p2, p2k = pkv[c]
                    T.add("dve", lambda e, p2=p2, c=c, hd=hd: e.scalar_tensor_tensor(
                        out=S[:, 2 * hd + c, :], in0=S[:, 2 * hd + c, :], scalar=g512[hd], in1=p2[:, :], op0=ALU.mult, op1=ALU.add),
                        reads=[p2k, ("S", hd)], writes=[("S", hd)])
            pscs = []
            for nt in range(4):
                psc, psck = ps_next()
                for mt in range(nt + 1):
                    mm_group(psc[:, mt * 128:(mt + 1) * 128],
                             [(qk[:, 2 + c, 128 * mt:128 * (mt + 1)], qk[:, c, 128 * nt:128 * (nt + 1)]) for c in range(2)],
                             ["qk_q", "qk_k"], psck)
                pscs.append((psc, psck))
            scs = []
            for nt in range(4):
                psc, psck = pscs[nt]
                sc, sck = sc_all[:, SC_OFF[nt]:SC_OFF[nt] + (nt + 1) * 128], ("sc", nt)
                w = (nt + 1) * 128
                T.add("dve", lambda e, psc=psc, sc=sc, hd=hd, nt=nt, w=w: e.tensor_tensor(
                    out=sc[:, 0:w].rearrange("p (a b) -> p a b", b=128), in0=psc[:, 0:w].rearrange("p (a b) -> p a b", b=128),
                    in1=maskT[:, hd, 3 - nt:4, :], op=ALU.mult), reads=[psck, "maskT"], writes=[sck])
                scs.append((sc, sck))
            pos = []
            for nt in range(4):
                sc, sck = scs[nt]
                po, pok = ps_next()
                ps_pinned.add(pok[1])
                mm_group(po[:, :], [(sc[:, mt * 128:(mt + 1) * 128], vv[:, mt, :]) for mt in range(nt + 1)]
                         + [(qp[:, c, 128 * nt:128 * (nt + 1)], Sbf[:, c, :]) for c in range(2)],
                         [sck, "qp", "Sbf"] + [("vv", mt) for mt in range(nt + 1)], pok)
                pos.append((po, pok))
            if not CONV_IN_F:
                pkv = []
                for c in range(2):
                    p2, p2k = ps_next()
                    ps_pinned.add(p2k[1])
                    mm_group(p2[:, :], [(kd[:, t, c * 128:(c + 1) * 128], vv[:, t, :]) for t in range(4)],
                             [("kd", t) for t in range(4)] + [("vv", t) for t in range(4)], p2k)
                    pkv.append((p2, p2k))
            else:
                conv_chunk(2 * hd)
                conv_chunk(2 * hd + 1)
            if HOIST:
                if hd < 3:
                    emit_v(hd + 1)
                else:
                    emit_gate_b()
            for nt in range(4):
                po, pok = pos[nt]
                T.add("dve", lambda e, po=po, nt=nt: e.bn_stats(out=st6[:, nt, :], in_=po[:, :]), reads=[pok], writes=[("st6", nt)])
                T.add("dve", lambda e, nt=nt: e.bn_aggr(out=mv[:, nt, :], in_=st6[:, nt, :]), reads=[("st6", nt)], writes=[("mv", nt)])
            mv_keys = [("mv", nt) for nt in range(4)]
            vA, vK = sm(4)
            T.add("dve", lambda e, vA=vA: e.tensor_scalar(out=vA, in0=mv[:, :, 1], scalar1=EPS, scalar2=None, op0=ALU.add),
                  reads=mv_keys, writes=[vK])
            sA, sK = sm(4)
            T.add("act", lambda e, vA=vA, sA=sA: e.activation(out=sA, in_=vA, func=AF.Sqrt), reads=[vK], writes=[sK])
            rA, rK = sm(4)
            T.add("dve", lambda e, sA=sA, rA=rA: e.reciprocal(out=rA, in_=sA), reads=[sK], writes=[rK])
            if not CONV_IN_F:
                for c in range(2):
                    p2, p2k = pkv[c]
                    T.add("dve", lambda e, p2=p2, c=c, hd=hd: e.scalar_tensor_tensor(
                        out=S[:, 2 * hd + c, :], in0=S[:, 2 * hd + c, :], scalar=g512[hd], in1=p2[:, :], op0=ALU.mult, op1=ALU.add),
                        reads=[p2k, ("S", hd)], writes=[("S", hd)])
                    ps_pinned.discard(p2k[1])
            ogs = []
            for nt in range(4):
                po, pok = pos[nt]
                tf, tfk = tf_rr.next()
                T.add("dve", lambda e, po=po, tf=tf, nt=nt: e.scalar_tensor_tensor(out=tf[:], in0=po[:, :], scalar=mv[:, nt, 0:1],
                                                                                  in1=gg[:, nt, :], op0=ALU.subtract, op1=ALU.mult),
                      reads=[pok, ("mv", nt), ("gg", nt)], writes=[tfk])
                ps_pinned.discard(pok[1])
                og, ogk = og_rr.next()
                T.add("act", lambda e, tf=tf, og=og, rA=rA, nt=nt: e.activation(out=og[:], in_=tf[:], func=AF.Copy,
                                                                               scale=rA[:, nt:nt + 1]),
                      reads=[tfk, rK], writes=[ogk])
                ogs.append((og, ogk))
            if HOISTQK and hd < 3:
                if HOISTV:
                    emit_qkA(hd + 1)
                    emit_v(hd + 1)
                    emit_qkB(hd + 1)
                    if HOISTG:
                        emit_g(hd + 1)
                else:
                    emit_qk(hd + 1)
                if HOISTALL:
                    emit_v(hd + 1)
                    if s == 0:
                        emit_sinit(hd + 1)
                    emit_g(hd + 1)
            if hd == 3 and PREFA and s + 1 < NS:
                prefA_ctx[0] = rms_part1(a_items(s + 1), npre=2)
            if HOISTGB and hd == 3:
                emit_gate_b()
            for nt in range(4):
                og, ogk = ogs[nt]
                tc = slice(128 * nt, 128 * (nt + 1))
                pt, ptk = pst_rr.next()

                def tro(e, pt=pt, og=og):
                    ins = None
                    for c in range(4):
                        ins = e.transpose(pt[:, c * 128:(c + 1) * 128], og[:, c * 128:(c + 1) * 128], ident[:])
                    return ins
                T.add("pe", tro, reads=[ogk, "ident"], writes=[ptk])
                def evo(e, pt=pt, hd=hd, tc=tc):
                    ins = None
                    for c in range(4):
                        ins = e.activation(out=oT[:, 4 * hd + c, tc], in_=pt[:, c * 128:(c + 1) * 128], func=AF.Copy,
                                           scale=gn_fm[:, 4 * hd + c:4 * hd + c + 1])
                    return ins
                T.add("act", evo, reads=[ptk, "gn_fm"], writes=[("oT", hd, nt)])
        if CONV_IN_F:
            stage_DE()
        oT_keys = [("oT", hd, t) for hd in range(4) for t in range(4)]
        if s == 0:
            dbg("oT0", oT[:], [128, 16, 512], BF16, oT_keys)
        if not HOIST and not HOISTGB:
            emit_gate_b()
        if PREFA and s + 1 < NS:
            load_rot(s + 1)
            rms_part2(prefA_ctx[0], GMIX)
        for gi in range(4):
            slot, wk = wload([(lambda sl: v16(sl)[:, :, :], w_o_v[:, :, 256 * gi:256 * (gi + 1)])])
            sv = v16(slot)
            for dd in range(2):
                d = 2 * gi + dd
                p, pk = ps_next()
                mm_group(p[:, :], [(sv[:, k, dd * 128:(dd + 1) * 128], oT[:, k, :]) for k in range(16)], [wk] + oT_keys, pk)
                tf, tfk = tf_rr.next()
                T.add("dve", lambda e, p=p, tf=tf, d=d: e.tensor_tensor(out=tf[:], in0=p[:, :], in1=cs[:, d, :], op=ALU.mult),
                      reads=[pk, ("cs", d)], writes=[tfk])
                T.add("dve", lambda e, tf=tf, d=d: e.tensor_tensor(out=ycg[:, d, :], in0=tf[:], in1=ycg[:, d, :], op=ALU.add),
                      reads=[tfk, ("ycg", d)], writes=[("ycg", d)])
        ycg_keys = [("ycg", d) for d in range(8)]
        if s == 0:
            dbg("ycgG0", ycg[:], [128, 8, 512], BF16, ycg_keys)
        for gi in range(2):
            slot, wk = load_cols(w_out_v, 512 * gi)
            sv = v8(slot)
            for t, i in enumerate(tiles):
                p, pk = ps_next()
                mm_group(p[:, :], [(ycg[:, k, 128 * t:128 * (t + 1)], sv[:, k, :]) for k in range(8)], [wk] + ycg_keys, pk)
                T.add("dve", lambda e, p=p, i=i, gi=gi: e.tensor_tensor(out=h[:, i, 512 * gi:512 * (gi + 1)], in0=p[:, :],
                                                                        in1=h[:, i, 512 * gi:512 * (gi + 1)], op=ALU.add),
                      reads=[pk, ("h", i)], writes=[("h", i)])

    dbg("hmix", h[:], [128, NT, D], F32, [("h", i) for i in range(NT)])
    T.barrier()
    print("sbuf bytes remaining (mixer phase):", nc.sbuf_bytes_remaining() if callable(nc.sbuf_bytes_remaining) else nc.sbuf_bytes_remaining)
    mst.close()

    u2T = sb("u2T", [128, 8, SEQ], BF16)
    wg = [sb("wg%d" % i, [128, 8, 512], BF16) for i in range(2)]
    wu = [sb("wu%d" % i, [128, 8, 512], BF16) for i in range(2)]
    wd = [sb("wd%d" % i, [128, 4, D], BF16) for i in range(2)]
    hid_rr = RR("hid", [sb("hid%d" % i, [128, 4, 512], BF16) for i in range(2)])
    tfm_rr = RR("tfm", [sb("tfm%d" % i, [128, 512]) for i in range(3)])
    comb = sb("comb", [128, NT, 16])
    wrt = sb("wrt", [128, 8, 20], BF16)
    brt = sb("brt", [128, 20])
    lg = sb("lg", [128, NT, 20])
    gfin = sb("gfin", [128, D])
    ob_rr = RR("ob", [sb("ob%d" % i, [128, D]) for i in range(2)])
    cur["u_rr"] = RR("u_tmB", [sb("u_tmB%d" % i, [128, D], BF16) for i in range(2)])
    r4 = [sb("r4_%d" % i, [128, NT, 4]) for i in range(8)]
    r1 = [sb("r1_%d" % i, [128, NT]) for i in range(10)]

    dma("pool", wrt[:], w_rt_d.rearrange("(kc p) n -> p kc n", p=128), [], ["wrt"], "m0")
    dma("sp", brt[:], brt_d[:, :], [], ["brt"], "c0")
    dma("sp", gfin[:], gfin_d[:, :], [], ["gfin"], "c1")

    def load_expert(e):
        b = e % 2
        dma("pool", wg[b][:], w_g_d[e].rearrange("(kc p) n -> p kc n", p=128), [], [("wg", b)], "wg%d" % b)
        dma("pool", wu[b][:], w_u_d[e].rearrange("(kc p) n -> p kc n", p=128), [], [("wu", b)], "wu%d" % b)
        dma("pool", wd[b][:], w_d_d[e].rearrange("(fc p) n -> p fc n", p=128), [], [("wd", b)], "wd%d" % b)

    load_expert(0)
    load_expert(1)

    for i in range(NT):
        if i % 4 == 0:
            rms_batch([(h[:, ii, :], ("h", ii), (lambda ii=ii: u2T[:, :, 128 * ii:128 * (ii + 1)]), ("u2T", ii)) for ii in range(i, i + 4)], GFFN)
        p, pk = ps_next()
        mm_group(p[:, 0:20], [(u2T[:, k, 128 * i:128 * (i + 1)], wrt[:, k, :]) for k in range(8)], [("u2T", i), "wrt"], pk)
        T.add("dve", lambda e, p=p, i=i: e.tensor_tensor(out=lg[:, i, :], in0=p[:, 0:20], in1=brt[:], op=ALU.add),
              reads=[pk, "brt"], writes=["lg"])

    def bc4(a):
        return a.unsqueeze(2).to_broadcast([128, NT, 4])

    def dv(fn, reads, writes):
        return T.add("dve", fn, reads=reads, writes=writes)

    gl = lg[:, :, 0:4]
    gmax, gsum, pg, m1, m2, e21, w1p, w2p, tmp1, tmp2 = [t[:] for t in r1]
    ohg, ge, sel, selt, oh1, oh2, sel2, cl = [t[:] for t in r4]
    dv(lambda e: e.tensor_reduce(out=gmax, in_=gl, axis=AX.X, op=ALU.max), ["lg"], ["gmax"])
    dv(lambda e: e.tensor_tensor(out=ge, in0=gl, in1=bc4(gmax), op=ALU.subtract), ["lg", "gmax"], ["ge"])
    T.add("act", lambda e: e.activation(out=ge, in_=ge, func=AF.Exp), reads=["ge"], writes=["ge"])
    dv(lambda e: e.tensor_reduce(out=gsum, in_=ge, axis=AX.X, op=ALU.add), ["ge"], ["gsum"])
    dv(lambda e: e.reciprocal(out=pg, in_=gsum), ["gsum"], ["pg"])
    dv(lambda e: e.tensor_tensor(out=ohg, in0=gl, in1=bc4(gmax), op=ALU.is_equal), ["lg", "gmax"], ["ohg"])
    for g in range(4):
        dst = sel if g == 0 else selt
        dv(lambda e, g=g, dst=dst: e.tensor_tensor(out=dst, in0=lg[:, :, 4 + 4 * g:8 + 4 * g],
                                                   in1=ohg[:, :, g:g + 1].to_broadcast([128, NT, 4]), op=ALU.mult),
           ["lg", "ohg"], ["sel" if g == 0 else "selt"])
        if g > 0:
            dv(lambda e: e.tensor_tensor(out=sel, in0=sel, in1=selt, op=ALU.add), ["sel", "selt"], ["sel"])
    dv(lambda e: e.tensor_reduce(out=m1, in_=sel, axis=AX.X, op=ALU.max), ["sel"], ["m1"])
    dv(lambda e: e.tensor_tensor(out=oh1, in0=sel, in1=bc4(m1), op=ALU.is_equal), ["sel", "m1"], ["oh1"])
    dv(lambda e: e.scalar_tensor_tensor(out=sel2, in0=oh1, scalar=-1e30, in1=sel, op0=ALU.mult, op1=ALU.add),
       ["oh1", "sel"], ["sel2"])
    dv(lambda e: e.tensor_reduce(out=m2, in_=sel2, axis=AX.X, op=ALU.max), ["sel2"], ["m2"])
    dv(lambda e: e.tensor_tensor(out=oh2, in0=sel2, in1=bc4(m2), op=ALU.is_equal), ["sel2", "m2"], ["oh2"])
    dv(lambda e: e.tensor_tensor(out=e21, in0=m2, in1=m1, op=ALU.subtract), ["m1", "m2"], ["e21"])
    T.add("act", lambda e: e.activation(out=e21, in_=e21, func=AF.Exp), reads=["e21"], writes=["e21"])
    dv(lambda e: e.tensor_scalar(out=tmp1, in0=e21, scalar1=1.0, scalar2=None, op0=ALU.add), ["e21"], ["tmp1"])
    dv(lambda e: e.reciprocal(out=tmp2, in_=tmp1), ["tmp1"], ["tmp2"])
    dv(lambda e: e.tensor_tensor(out=w1p, in0=tmp2, in1=pg, op=ALU.mult), ["tmp2", "pg"], ["w1p"])
    dv(lambda e: e.tensor_tensor(out=w2p, in0=w1p, in1=e21, op=ALU.mult), ["w1p", "e21"], ["w2p"])
    dv(lambda e: e.tensor_tensor(out=cl, in0=oh1, in1=bc4(w1p), op=ALU.mult), ["oh1", "w1p"], ["cl"])
    dv(lambda e: e.tensor_tensor(out=selt, in0=oh2, in1=bc4(w2p), op=ALU.mult), ["oh2", "w2p"], ["selt"])
    dv(lambda e: e.tensor_tensor(out=cl, in0=cl, in1=selt, op=ALU.add), ["cl", "selt"], ["cl"])
    for g in range(4):
        dv(lambda e, g=g: e.tensor_tensor(out=comb[:, :, 4 * g:4 * g + 4], in0=cl,
                                          in1=ohg[:, :, g:g + 1].to_broadcast([128, NT, 4]), op=ALU.mult),
           ["cl", "ohg"], [("comb", g)])
    comb_keys = [("comb", g) for g in range(4)]
    dbg("u2T", u2T[:], [128, 8, SEQ], BF16, [("u2T", i) for i in range(NT)])
    dbg("lg", lg[:], [128, NT, 20], F32, ["lg"])
    dbg("comb", comb[:], [128, NT, 16], F32, comb_keys)

    out_ops = []

    def final_tiles(s):
        for i in range(4 * s, 4 * s + 4):
            junk, jk = cur["u_rr"].next()
            ssA, ssK = sm()
            T.add("act", lambda e, i=i, ssA=ssA, junk=junk: e.activation(out=junk[:], in_=h[:, i, :], func=AF.Square, accum_out=ssA),
                  reads=[("h", i)], writes=[ssK, jk])
            rsA, rsK = sm()
            T.add("dve", lambda e, ssA=ssA, rsA=rsA: e.tensor_scalar(out=rsA, in0=ssA, scalar1=1.0 / D, scalar2=EPS, op0=ALU.mult, op1=ALU.add),
                  reads=[ssK], writes=[rsK])
            sdA, sdK = sm()
            T.add("act", lambda e, rsA=rsA, sdA=sdA: e.activation(out=sdA, in_=rsA, func=AF.Sqrt), reads=[rsK], writes=[sdK])
            rA, rK = sm()
            T.add("dve", lambda e, sdA=sdA, rA=rA: e.reciprocal(out=rA, in_=sdA), reads=[sdK], writes=[rK])
            ob, obk = ob_rr.next()
            T.add("dve", lambda e, i=i, rA=rA, ob=ob: e.scalar_tensor_tensor(out=ob[:], in0=h[:, i, :], scalar=rA, in1=gfin[:],
                                                                            op0=ALU.mult, op1=ALU.mult),
                  reads=[("h", i), rK, "gfin"], writes=[obk])
            out_ops.append(dma("sp", out_d[128 * i:128 * (i + 1), :], ob[:], [obk], [("out", i)], "o%d" % (i % 2)))

    def gate_up(ex, s):
        b = ex % 2
        hid, hidk = hid_rr.next()
        u2_keys = [("u2T", 4 * s + t) for t in range(4)]
        for f in range(4):
            pgt, pgk = ps_next()
            mm_group(pgt[:, :], [(wg[b][:, k, f * 128:(f + 1) * 128], u2T[:, k, 512 * s:512 * (s + 1)]) for k in range(8)],
                     [("wg", b)] + u2_keys, pgk)
            put, puk = ps_next()
            mm_group(put[:, :], [(wu[b][:, k, f * 128:(f + 1) * 128], u2T[:, k, 512 * s:512 * (s + 1)]) for k in range(8)],
                     [("wu", b)] + u2_keys, puk)
            tf, tfk = tfm_rr.next()
            T.add("act", lambda e, pgt=pgt, tf=tf: e.activation(out=tf[:], in_=pgt[:, :], func=AF.Silu), reads=[pgk], writes=[tfk])
            T.add("dve", lambda e, put=put, tf=tf, hid=hid, f=f: e.tensor_tensor(out=hid[:, f, :], in0=put[:, :], in1=tf[:], op=ALU.mult),
                  reads=[puk, tfk], writes=[(hidk, f)])
        return hid, hidk

    def down(ex, s, hid, hidk):
        b = ex % 2
        hid_keys = [(hidk, f) for f in range(4)]
        for t in range(4):
            i = 4 * s + t
            for half in range(2):
                py, pyk = ps_next()
                mm_group(py[:, :], [(hid[:, f, 128 * t:128 * (t + 1)], wd[b][:, f, 512 * half:512 * (half + 1)]) for f in range(4)],
                         [("wd", b)] + hid_keys, pyk)
                T.add("dve", lambda e, py=py, i=i, half=half, ex=ex: e.scalar_tensor_tensor(
                    out=h[:, i, 512 * half:512 * (half + 1)], in0=py[:, :], scalar=comb[:, i, ex:ex + 1],
                    in1=h[:, i, 512 * half:512 * (half + 1)], op0=ALU.mult, op1=ALU.add),
                    reads=[pyk, ("h", i)] + comb_keys, writes=[("h", i)])
        if ex == NE - 1:
            final_tiles(s)
        if s == NS - 1 and ex + 2 < NE:
            load_expert(ex + 2)

    units = [(ex, s) for ex in range(NE) for s in range(NS)]
    if MOE_PIPE:
        prev = gate_up(*units[0])
        for u in range(len(units)):
            nxt = gate_up(*units[u + 1]) if u + 1 < len(units) else None
            down(units[u][0], units[u][1], *prev)
            prev = nxt
    else:
        for (ex, s) in units:
            hk = gate_up(ex, s)
            down(ex, s, *hk)

    T.add("sp", None, extra=out_ops + dbg_ops)

    with nc.Block() as block:
        T.emit(block)
    return nc


def _constants():
    f32 = np.float32
    half = 128
    inv = (f32(10000.0) ** (-(np.arange(half, dtype=f32)) / f32(half))).astype(f32)
    pos = np.arange(SEQ + NMETA, dtype=f32)
    ang = (pos[None, :] * inv[:, None]).astype(f32)
    cos_t = np.cos(ang).astype(f32)
    sin_t = np.sin(ang).astype(f32)
    lgam = np.log(f32(1.0) - f32(2.0) ** (-f32(5.0) - np.arange(4, dtype=f32))).astype(f32)
    m = np.arange(128, dtype=f32)[:, None]
    n = np.arange(128, dtype=f32)[None, :]
    same = (np.floor(m / 64) == np.floor(n / 64))
    causal_cross = (n >= 64) & (m < 64)
    maskT = np.zeros((128, 4, 4, 128), f32)
    qdec = np.zeros((128, 4, 128), f32)
    kdec = np.zeros((128, 4), f32)
    kdecm = np.zeros((NMETA, 4), f32)
    for hh in range(4):
        dec = np.exp(lgam[hh] * np.abs(n - m)).astype(f32)
        mk = np.where(same | causal_cross, dec, f32(0.0)).astype(f32)
        maskT[:, hh, 3, :] = mk * f32(1.0 / 16.0)
        for dd in range(1, 4):
            maskT[:, hh, 3 - dd, :] = np.exp(lgam[hh] * (f32(128.0 * dd) + n - m)).astype(f32) * f32(1.0 / 16.0)
        qdec[:, hh, :] = np.exp(lgam[hh] * (np.arange(128, dtype=f32) + 1.0)).astype(f32)[None, :]
        kdec[:, hh] = np.exp(lgam[hh] * (127.0 - np.arange(128, dtype=f32))).astype(f32) * f32(1.0 / 16.0)
        kdecm[:, hh] = np.exp(lgam[hh] * (15.0 - np.arange(NMETA, dtype=f32))).astype(f32) * f32(1.0 / 16.0)
    ident = np.eye(128, dtype=f32).astype(ml_dtypes.bfloat16)
    return dict(ident=ident, cos_t=cos_t, sin_t=sin_t, maskT=maskT, qdec=qdec, kdec=kdec, kdec_meta=kdecm)


_CACHE = {}


def kernel(x, meta_tokens, norm_mix_g, w_in, conv_dw_w, conv_dw_b, conv_ln_g, conv_ln_b,
           conv_pw_w, ret_gn_g, ret_w_o, w_out, norm_ffn_g, w_group_router, b_group_router,
           w_expert_router, b_expert_router, w_expert_gate, w_expert_up, w_expert_down,
           norm_final_g):
    f32 = np.float32
    A = lambda a: np.ascontiguousarray(np.asarray(a, dtype=f32))
    x = A(x)

    def fm(v):
        return np.asarray(v, f32).reshape(8, 128).T

    fmvec = np.zeros((128, 6, 8), f32)
    fmvec[:, 0] = fm(norm_mix_g[0])
    fmvec[:, 1] = fm(norm_ffn_g[0])
    fmvec[:, 2] = fm(conv_dw_b[0])
    fmvec[:, 3] = fm(conv_ln_g[0])
    fmvec[:, 4] = fm(conv_ln_b[0])
    convw_fm = A(np.asarray(conv_dw_w[0], f32).reshape(CW, 8, 128).transpose(2, 1, 0))
    gn_fm = A(np.asarray(ret_gn_g[0], f32).reshape(16, 128).T)
    w_rt = A(np.concatenate([np.asarray(w_group_router[0], f32), np.asarray(w_expert_router[0], f32)], axis=1))
    b_rt = A(np.broadcast_to(np.concatenate([np.asarray(b_group_router[0], f32), np.asarray(b_expert_router[0], f32)])[None, :], (128, 20)))
    gfin_b = A(np.broadcast_to(np.asarray(norm_final_g, f32)[None, :], (128, D)))
    shared = dict(
        meta=A(meta_tokens), w_in=A(w_in[0]), w_pw=A(conv_pw_w[0]), w_o=A(ret_w_o[0]), w_out=A(w_out[0]),
        w_rt=w_rt, w_gate=A(w_expert_gate[0]), w_up=A(w_expert_up[0]), w_down=A(w_expert_down[0]),
        fmvec=A(fmvec), convw_fm=convw_fm, gn_fm=gn_fm, b_rt=b_rt, gfin_b=gfin_b,
    )
    shared.update(_constants())
    if "nc" not in _CACHE:
        _CACHE["nc"] = build_program()
    nc = _CACHE["nc"]
    in_maps = []
    for b in range(8):
        m = dict(shared)
        m["x"] = np.ascontiguousarray(x[b])
        in_maps.append(m)
    res = run_bass_kernel_spmd(nc, in_maps, core_ids=list(range(8)))
    out = np.stack([np.asarray(res.results[b]["out"], dtype=f32) for b in range(8)], axis=0)
    return out
```

```python
import numpy as np
from contextlib import ExitStack
import ml_dtypes
import concourse.bass as bass
import concourse.mybir as mybir
from concourse.bass_utils import run_bass_kernel_spmd

F32 = mybir.dt.float32
BF16 = mybir.dt.bfloat16
AF = mybir.ActivationFunctionType
ALU = mybir.AluOpType
AX = mybir.AxisListType

D = 1024
SEQ = 2048
NT = 16
NS = 4
NMETA = 16
D_IN = 10240
CW = 31
EPS = 1e-6
NE = 16
SC_OFF = (0, 128, 384, 768)
OFF_Q, OFF_K, OFF_V, OFF_G, OFF_GA, OFF_GB = 2048, 3072, 4096, 6144, 8192, 9216

ENGS = ("pe", "act", "dve", "pool", "sp")
UNIFIED_PSUM = True
HOIST = False
SYNC_WAW = True
PRE0 = True
MOE_PIPE = True
HOISTQK = True
PREQKA = True
HOISTV = True
PREDIAG = True
PREFA = False
PREQK = False
HOISTALL = False
HOISTGB = True
ROT_ENG = "dve"
CONV_IN_F = False
SYNC_ALL = True
DIAG_DVE = True


class Op:
    __slots__ = ("eng", "fn", "is_dma", "sem", "val", "seq", "inc", "cnt", "waits", "know")


class Tracker:
    def __init__(self, nc):
        self.nc = nc
        self.streams = {e: [] for e in ENGS}
        self.keys = {}
        self.eng_know = {e: {} for e in ENGS}
        self.dma_sems = {}
        self.comp_sem = {}
        self.nops = 0

    @staticmethod
    def _merge(a, b):
        for k, v in b.items():
            if a.get(k, -1) < v:
                a[k] = v

    def add(self, eng, fn, reads=(), writes=(), dma=None, extra=()):
        op = Op()
        op.eng, op.fn, op.is_dma, op.inc, op.cnt = eng, fn, dma is not None, False, 0
        op.seq = len(self.streams[eng])
        deps = []
        for k in reads:
            st = self.keys.get(k)
            if st is not None and st[0] is not None:
                deps.append((st[0], "raw"))
        for k in writes:
            st = self.keys.get(k)
            if st is not None:
                if st[0] is not None:
                    deps.append((st[0], "waw"))
                for r in st[1].values():
                    deps.append((r, "war"))
        for d in extra:
            deps.append((d, "raw"))
        if dma is not None:
            ds = self.dma_sems.get(dma)
            if ds is None:
                ds = [self.nc.alloc_semaphore("dsem_%d" % len(self.dma_sems)), 0, None]
                self.dma_sems[dma] = ds
            if ds[2] is not None:
                deps.append((ds[2], "raw"))
            ds[1] += 16
            op.sem, op.val = dma, ds[1]
            ds[2] = op
        else:
            op.sem, op.val = None, 0
        know = dict(self.eng_know[eng])
        waits = []
        deps.sort(key=lambda t: -(t[0].val if t[0].is_dma else t[0].seq))
        for d, kind in deps:
            if d is op:
                continue
            if d.is_dma:
                kk = ("d", d.sem)
                if know.get(kk, 0) >= d.val:
                    continue
                waits.append(d)
                self._merge(know, d.know)
                know[kk] = d.val
            else:
                if d.eng == eng and not op.is_dma:
                    if eng == "pe" or ((kind == "war" or (kind == "waw" and not SYNC_WAW)) and not SYNC_ALL):
                        continue
                if know.get(d.eng, -1) >= d.seq:
                    continue
                waits.append(d)
                d.inc = True
                self._merge(know, d.know)
                know[d.eng] = d.seq
        op.waits, op.know = waits, know
        self.eng_know[eng] = know
        self.streams[eng].append(op)
        for k in reads:
            st = self.keys.get(k)
            if st is None:
                st = [None, {}]
                self.keys[k] = st
            st[1][("d", op.sem) if op.is_dma else eng] = op
        for k in writes:
            self.keys[k] = [op, {}]
        self.nops += 1
        return op

    def lasts(self):
        out = []
        for e in ENGS:
            for op in reversed(self.streams[e]):
                if (not op.is_dma) and op.fn is not None:
                    out.append(op)
                    break
        for ds in self.dma_sems.values():
            if ds[2] is not None:
                out.append(ds[2])
        return out

    def barrier(self):
        ls = self.lasts()
        for e in ENGS:
            self.add(e, None, extra=ls)

    def emit(self, block):
        nc = self.nc
        for e in ("pe", "act", "dve", "pool"):
            self.comp_sem[e] = nc.alloc_semaphore("csem_" + e)
        for e in ENGS:
            c = 0
            for op in self.streams[e]:
                if op.inc:
                    c += 1
                op.cnt = c

        def run(ename, eng):
            for op in self.streams[ename]:
                for d in op.waits:
                    if d.is_dma:
                        eng.wait_ge(self.dma_sems[d.sem][0], d.val)
                    else:
                        eng.wait_ge(self.comp_sem[d.eng], d.cnt)
                if op.fn is None:
                    continue
                ins = op.fn(eng)
                if op.is_dma:
                    ins.then_inc(self.dma_sems[op.sem][0], 16)
                elif op.inc:
                    ins.then_inc(self.comp_sem[ename], 1)

        @block.tensor
        def _(eng):
            run("pe", eng)

        @block.scalar
        def _(eng):
            run("act", eng)

        @block.vector
        def _(eng):
            run("dve", eng)

        @block.gpsimd
        def _(eng):
            run("pool", eng)

        @block.sync
        def _(eng):
            run("sp", eng)


class RR:
    def __init__(self, name, bufs):
        self.name, self.bufs, self.i = name, bufs, 0

    def next(self):
        i = self.i
        self.i = (i + 1) % len(self.bufs)
        return self.bufs[i], (self.name, i)


def build_program(debug=False):
    nc = bass.Bass("TRN2", target_bir_lowering=False)
    T = Tracker(nc)

    def din(name, shape, dt=F32):
        return nc.dram_tensor(name, list(shape), dt, kind="ExternalInput").ap()

    x_d = din("x", [SEQ, D])
    meta_d = din("meta", [NMETA, D])
    w_in_d = din("w_in", [D, D_IN])
    w_pw_d = din("w_pw", [D, D])
    w_o_d = din("w_o", [2048, D])
    w_out_d = din("w_out", [D, D])
    w_rt_d = din("w_rt", [D, 20])
    w_g_d = din("w_gate", [NE, D, 512])
    w_u_d = din("w_up", [NE, D, 512])
    w_d_d = din("w_down", [NE, 512, D])
    ident_d = din("ident", [128, 128], BF16)
    cos_d = din("cos_t", [128, SEQ + NMETA])
    sin_d = din("sin_t", [128, SEQ + NMETA])
    mask_d = din("maskT", [128, 4, 4, 128])
    qdec_d = din("qdec", [128, 4, 128])
    kdec_d = din("kdec", [128, 4])
    kdecm_d = din("kdec_meta", [NMETA, 4])
    fmv_d = din("fmvec", [128, 6, 8])
    convw_d = din("convw_fm", [128, 8, CW])
    gn_d = din("gn_fm", [128, 16])
    brt_d = din("b_rt", [128, 20])
    gfin_d = din("gfin_b", [128, D])
    out_d = nc.dram_tensor("out", [SEQ, D], F32, kind="ExternalOutput").ap()
    dscr = nc.dram_tensor("diag_scr", [8, 128, CW * 128], BF16, kind="Internal").ap()

    def sb(name, shape, dt=F32):
        return nc.alloc_sbuf_tensor("s_" + name, list(shape), dt)

    dbg_ops = []

    def dbg(name, ap, shape, dt, keys):
        if not debug:
            return
        d = nc.dram_tensor("dbg_" + name, list(shape), dt, kind="ExternalOutput").ap()
        dbg_ops.append(T.add("sp", lambda e: e.dma_start(out=d, in_=ap), reads=keys, writes=[("dbg", name)], dma="dbg_" + name))

    def psum(name, shape, dt=F32):
        return nc.alloc_psum_tensor(name, list(shape), dt)

    h = sb("h", [128, NT, D])
    ident = sb("ident_sb", [128, 128], BF16)
    fmv = sb("fmv", [128, 6, 8])
    small = sb("small", [128, 64])
    small_i = [0]

    def sm(n=1):
        i = small_i[0]
        small_i[0] = (i + 4) % 64
        return small[:, i:i + n], ("small", i // 4)

    if UNIFIED_PSUM:
        NPS = 8
        psf = [psum("psf%d" % i, [128, 512]) for i in range(NPS)]
    else:
        NPS = 6
        psf = [psum("psf%d" % i, [128, 512]) for i in range(NPS)]
        pst = [psum("pst%d" % i, [128, 1024], BF16) for i in range(2)]
    ps_i = [0]
    ps_pinned = set()

    def ps_next():
        while True:
            i = ps_i[0]
            ps_i[0] = (i + 1) % NPS
            if i not in ps_pinned:
                return psf[i], ("psf", i)

    class _PstRR:
        def next(self):
            bank, key = ps_next()
            return bank[:, :].bitcast(BF16), key

    pst_rr = _PstRR() if UNIFIED_PSUM else RR("pst", pst)

    def dma(queue, out, in_, reads, writes, sem):
        return T.add(queue, lambda e: e.dma_start(out=out, in_=in_), reads=reads, writes=writes, dma=sem)

    dma("sp", ident[:], ident_d[:, :], [], ["ident"], "c0")
    dma("sp", fmv[:], fmv_d[:, :, :], [], ["fmv"], "c1")

    GMIX, GFFN, CONVB, LNG, LNB = 0, 1, 2, 3, 4

    mst = ExitStack()
    cur = {}

    def msb(name, shape, dt=F32):
        return mst.enter_context(nc.sbuf_tensor("m_" + name, list(shape), dt))

    wslots = [msb("wslot%d" % i, [128, 4096], BF16) for i in range(3)]
    w_rr = RR("wslot", wslots)
    uT = msb("uT", [128, 8, 512], BF16)
    uTm = msb("uTm", [128, 8, NMETA], BF16)
    hc = msb("hc", [128, 8, 542], BF16)
    cs = msb("cs", [128, 8, 512], BF16)
    ycg = msb("ycg", [128, 8, 512], BF16)
    qk = msb("qk", [128, 4, 512], BF16)
    qp = msb("qp", [128, 2, 512], BF16)
    kd = msb("kd", [128, 4, 256], BF16)
    vv = msb("vv", [128, 4, 512], BF16)
    gg = msb("gg", [128, 4, 512], BF16)
    oT = msb("oT", [128, 16, 512], BF16)
    S = msb("S", [128, 8, 512])
    Sbf = msb("Sbf", [128, 2, 512], BF16)
    cos_sb = msb("cos_sb", [128, 512])
    sin_sb = msb("sin_sb", [128, 512])
    cosm = msb("cosm", [128, NMETA])
    sinm = msb("sinm", [128, NMETA])
    maskT = msb("maskT", [128, 4, 4, 128], BF16)
    qdec = msb("qdec", [128, 4, 128])
    kdec = msb("kdec", [128, 4])
    kdecm = msb("kdecm", [NMETA, 4])
    convw = msb("convw", [128, 8, CW])
    gn_fm = msb("gn_fm", [128, 16])
    ones_bf = msb("ones_bf", [128, 128], BF16)
    tf_rr = RR("tf", [msb("tf%d" % i, [128, 512]) for i in range(3)])
    sq_rr = RR("sq", [msb("sq%d" % i, [128, 512], BF16) for i in range(2)])
    cur["u_rr"] = RR("u_tm", [msb("u_tm%d" % i, [128, D], BF16) for i in range(2)])
    sc_all = msb("sc_all", [128, 1280], BF16)
    og_rr = RR("og", [msb("og%d" % i, [128, 512], BF16) for i in range(4)])
    kmeta = msb("kmeta", [128, 2, NMETA], BF16)
    kdm = msb("kdm", [NMETA, 256], BF16)
    vm = msb("vm", [NMETA, 512], BF16)
    st6 = msb("st6", [128, 4, 6])
    stat2 = msb("stat2", [128, 2, 512])
    mu_b = stat2[:, 0, :]
    rs_b = stat2[:, 1, :]
    xm = stat2[0:NMETA, :, :].rearrange("p a b -> p (a b)")
    mv = msb("mv", [128, 4, 2])

    dma("pool", maskT[:], mask_d[:, :, :, :], [], ["maskT"], "m0")
    dma("sp", qdec[:], qdec_d[:, :, :], [], ["qdec"], "c3")
    dma("sp", kdec[:], kdec_d[:, :], [], ["kdec"], "c0")
    dma("sp", kdecm[:], kdecm_d[:, :], [], ["kdecm"], "c1")
    dma("sp", convw[:], convw_d[:, :, :], [], ["convw"], "c2")
    dma("sp", gn_fm[:], gn_d[:, :], [], ["gn_fm"], "c3")
    dma("sp", cosm[:], cos_d[:, 0:NMETA], [], ["cosm"], "c0")
    dma("sp", sinm[:], sin_d[:, 0:NMETA], [], ["sinm"], "c1")
    T.add("dve", lambda e: e.memset(ones_bf[:], 1.0), writes=["ones"])
    T.add("dve", lambda e: e.memset(hc[:, :, 0:14], 0.0), writes=["hc_hist"])

    gam = [1.0 - 2.0 ** (-5.0 - i) for i in range(4)]
    lgam = [float(np.log(np.float32(1.0) - np.float32(2.0) ** np.float32(-5.0 - i))) for i in range(4)]

    def gpow(hd, e):
        return float(np.exp(lgam[hd] * e))

    g512 = [gpow(hd, 512) for hd in range(4)]

    def wload(parts):
        slot, key = w_rr.next()
        for k, (dstf, src) in enumerate(parts):
            dma("pool", dstf(slot), src, [], [key], "w%d" % key[1])
        return slot, key

    w_in_v = w_in_d.rearrange("(kc p) n -> p kc n", p=128)
    w_pw_v = w_pw_d.rearrange("(kc p) n -> p kc n", p=128)
    w_o_v = w_o_d.rearrange("(kc p) n -> p kc n", p=128)
    w_out_v = w_out_d.rearrange("(kc p) n -> p kc n", p=128)

    def v8(slot):
        return slot[:].rearrange("p (k c) -> p k c", c=512)

    def v16(slot):
        return slot[:].rearrange("p (k c) -> p k c", c=256)

    def load_cols(view, c0, n=512):
        return wload([(lambda s: v8(s)[:, :, 0:n], view[:, :, c0:c0 + n])])

    def load_cols2(view, ca, cb):
        return wload([(lambda s: v8(s)[:, :, 0:256], view[:, :, ca:ca + 256]),
                      (lambda s: v8(s)[:, :, 256:512], view[:, :, cb:cb + 256])])

    def rms_to_fm(src_ap, npart, dst_fn, src_key, dst_key, gsel):
        u_tm, uk = cur["u_rr"].next()
        skeys = src_key if isinstance(src_key, list) else [src_key]
        ssA, ssK = sm()
        T.add("act", lambda e: e.activation(out=u_tm[0:npart, :], in_=src_ap, func=AF.Square, accum_out=ssA[0:npart, :]),
              reads=skeys, writes=[ssK, uk])
        rsA, rsK = sm()
        T.add("dve", lambda e: e.tensor_scalar(out=rsA[0:npart, :], in0=ssA[0:npart, :], scalar1=1.0 / D, scalar2=EPS,
                                               op0=ALU.mult, op1=ALU.add), reads=[ssK], writes=[rsK])
        sdA, sdK = sm()
        T.add("act", lambda e: e.activation(out=sdA[0:npart, :], in_=rsA[0:npart, :], func=AF.Sqrt), reads=[rsK], writes=[sdK])
        rA, rK = sm()
        T.add("dve", lambda e: e.reciprocal(out=rA[0:npart, :], in_=sdA[0:npart, :]), reads=[sdK], writes=[rK])
        T.add("dve", lambda e: e.tensor_scalar(out=u_tm[0:npart, :], in0=src_ap, scalar1=rA[0:npart, :], scalar2=None,
                                               op0=ALU.mult), reads=skeys + [rK], writes=[uk])
        pt, ptk = pst_rr.next()

        def tr(e):
            ins = None
            for k in range(8):
                ins = e.transpose(pt[:, k * npart:(k + 1) * npart], u_tm[0:npart, k * 128:(k + 1) * 128],
                                  ident[0:npart, 0:npart])
            return ins
        T.add("pe", tr, reads=[uk, "ident"], writes=[ptk])
        T.add("dve", lambda e: e.tensor_tensor(
            out=dst_fn(), in0=pt[:, 0:8 * npart].rearrange("p (k n) -> p k n", n=npart),
            in1=fmv[:, gsel, :].unsqueeze(2).to_broadcast([128, 8, npart]), op=ALU.mult),
            reads=[ptk, "fmv"], writes=[dst_key])

    def rms_part1(items, npre=0):
        n = len(items)
        ssA, ssK = sm(4)
        for t, (src_ap, src_key, dst_fn, dst_key) in enumerate(items):
            junk, jk = cur["u_rr"].next()
            T.add("act", lambda e, junk=junk, src_ap=src_ap, t=t: e.activation(out=junk[:], in_=src_ap, func=AF.Square,
                                                                              accum_out=ssA[:, t:t + 1]),
                  reads=[src_key], writes=[(ssK, t), jk])
        ss_keys = [(ssK, t) for t in range(n)]
        rsA, rsK = sm(4)
        T.add("dve", lambda e: e.tensor_scalar(out=rsA[:, 0:n], in0=ssA[:, 0:n], scalar1=1.0 / D, scalar2=EPS,
                                               op0=ALU.mult, op1=ALU.add), reads=ss_keys, writes=[rsK])
        sdA, sdK = sm(4)
        T.add("act", lambda e: e.activation(out=sdA[:, 0:n], in_=rsA[:, 0:n], func=AF.Sqrt), reads=[rsK], writes=[sdK])
        rA, rK = sm(4)
        T.add("dve", lambda e: e.reciprocal(out=rA[:, 0:n], in_=sdA[:, 0:n]), reads=[sdK], writes=[rK])
        pre = []
        for t in range(npre):
            pre.append(rms_scale(items[t], t, rA, rK))
        return dict(items=items, rA=rA, rK=rK, pre=pre)

    def rms_scale(item, t, rA, rK):
        src_ap, src_key, dst_fn, dst_key = item
        u_tm, uk = cur["u_rr"].next()
        T.add("act", lambda e: e.activation(out=u_tm[:], in_=src_ap, func=AF.Copy, scale=rA[:, t:t + 1]),
              reads=[src_key, rK], writes=[uk])
        return u_tm, uk

    def rms_part2(ctx, gsel):
        items, rA, rK = ctx["items"], ctx["rA"], ctx["rK"]
        for t, item in enumerate(items):
            src_ap, src_key, dst_fn, dst_key = item
            u_tm, uk = ctx["pre"][t] if t < len(ctx["pre"]) else rms_scale(item, t, rA, rK)
            pt, ptk = pst_rr.next()

            def tr(e, pt=pt, u_tm=u_tm):
                ins = None
                for k in range(8):
                    ins = e.transpose(pt[:, k * 128:(k + 1) * 128], u_tm[:, k * 128:(k + 1) * 128], ident[:])
                return ins
            T.add("pe", tr, reads=[uk, "ident"], writes=[ptk])
            T.add("dve", lambda e, pt=pt, dst_fn=dst_fn: e.tensor_tensor(
                out=dst_fn(), in0=pt[:, :].rearrange("p (k n) -> p k n", n=128),
                in1=fmv[:, gsel, :].unsqueeze(2).to_broadcast([128, 8, 128]), op=ALU.mult),
                reads=[ptk, "fmv"], writes=[dst_key])

    def rms_batch(items, gsel):
        rms_part2(rms_part1(items), gsel)

    def mm_group(out_ap, pairs, reads, wkey):
        n = len(pairs)

        def fn(e):
            ins = None
            for i, (l, r) in enumerate(pairs):
                ins = e.matmul(out_ap, lhsT=l, rhs=r, start=(i == 0), stop=(i == n - 1))
            return ins
        return T.add("pe", fn, reads=reads, writes=[wkey])

    prefA_ctx = [None]
    prediag = {}
    for s in range(NS):
        c0 = NMETA + 512 * s
        tiles = range(4 * s, 4 * s + 4)
        uT_keys = [("uT", t) for t in range(4)]
        def a_items(ss):
            return [(h[:, 4 * ss + t, :], ("h", 4 * ss + t), (lambda t=t: uT[:, :, 128 * t:128 * (t + 1)]), ("uT", t)) for t in range(4)]

        def load_x(ss):
            for i in range(4 * ss, 4 * ss + 4):
                dma("sp", h[:, i, :], x_d[128 * i:128 * (i + 1), :], [], [("h", i)], "x%d" % (i % 4))

        def load_rot(ss):
            cc = NMETA + 512 * ss
            dma("sp", cos_sb[:], cos_d[:, cc:cc + 512], [], ["cos"], "c2")
            dma("sp", sin_sb[:], sin_d[:, cc:cc + 512], [], ["sin"], "c3")

        if s == 0 or not PREFA:
            load_rot(s)
            if s == 0:
                dma("sp", xm, meta_d[:, :], [], ["mu_b", "rs_b"], "xm")
                rms_to_fm(xm, NMETA, lambda: uTm[:, :, :], ["mu_b", "rs_b"], "uTm", GMIX)
            load_x(s)
            rms_batch(a_items(s), GMIX)
        if PREFA and s + 1 < NS:
            load_x(s + 1)

        if s == 0:
            dbg("uT0", uT[:], [128, 8, 512], BF16, uT_keys)
            dbg("uTm", uTm[:], [128, 8, NMETA], BF16, ["uTm"])
        if s > 0:
            T.add("dve", lambda e: e.tensor_copy(out=hc[:, :, 0:30], in_=hc[:, :, 512:542]),
                  reads=[("hc", j) for j in range(8)], writes=["hc_hist"])
        for cg in range(4):
            slot, wk = load_cols2(w_in_v, 256 * cg, 1024 + 256 * cg)
            sv = v8(slot)
            for jj in range(2):
                j = 2 * cg + jj
                pa, pak = ps_next()
                mm_group(pa[:, :], [(sv[:, k, jj * 128:(jj + 1) * 128], uT[:, k, :]) for k in range(8)],
                         [wk] + uT_keys, pak)
                pg, pgk = ps_next()
                mm_group(pg[:, :], [(sv[:, k, 256 + jj * 128:256 + (jj + 1) * 128], uT[:, k, :]) for k in range(8)],
                         [wk] + uT_keys, pgk)
                tf, tfk = tf_rr.next()
                T.add("act", lambda e, pg=pg, tf=tf: e.activation(out=tf[:], in_=pg[:, :], func=AF.Sigmoid),
                      reads=[pgk], writes=[tfk])
                T.add("dve", lambda e, pa=pa, tf=tf, j=j: e.tensor_tensor(out=hc[:, j, 30:542], in0=pa[:, :], in1=tf[:], op=ALU.mult),
                      reads=[pak, tfk], writes=[("hc", j)])
                if s == 0:
                    pa2, pak2 = ps_next()
                    mm_group(pa2[:, 0:NMETA], [(sv[:, k, jj * 128:(jj + 1) * 128], uTm[:, k, :]) for k in range(8)],
                             [wk, "uTm"], pak2)
                    mm_group(pa2[:, 32:32 + NMETA], [(sv[:, k, 256 + jj * 128:256 + (jj + 1) * 128], uTm[:, k, :]) for k in range(8)],
                             [wk, "uTm"], pak2)
                    tf2, tfk2 = tf_rr.next()
                    T.add("act", lambda e, p=pa2, tf=tf2: e.activation(out=tf[:, 0:NMETA], in_=p[:, 32:32 + NMETA], func=AF.Sigmoid),
                          reads=[pak2], writes=[tfk2])
                    T.add("dve", lambda e, p=pa2, tf=tf2, j=j: e.tensor_tensor(out=hc[:, j, 14:30], in0=p[:, 0:NMETA], in1=tf[:, 0:NMETA], op=ALU.mult),
                          reads=[pak2, tfk2], writes=[("hcm", j)])

        if s == 0:
            dbg("hc0", hc[:], [128, 8, 542], BF16, [("hc", j) for j in range(8)] + [("hcm", j) for j in range(8)] + ["hc_hist"])
        if s == 0 and PREDIAG:
            for j in range(6):
                hv = h[:, 4 + 2 * j:6 + 2 * j, :].rearrange("p a b -> p (a b)").bitcast(BF16)
                dv3 = hv[:, 0:CW * 128].rearrange("p (t c) -> p t c", c=128)
                hk = [("h", 4 + 2 * j), ("h", 5 + 2 * j)]
                seen = set()
                for tap in range(CW):
                    who = ("dve", "act", "pool", "act", "dve", "act", "dve", "act")[tap % 8]
                    first = who not in seen
                    seen.add(who)
                    wr = (hk if first else []) + [("pd", j, who)]
                    if who == "act":
                        T.add("act", lambda e, dv3=dv3, j=j, tap=tap: e.activation(out=dv3[:, tap, :], in_=ident[:], func=AF.Copy,
                                                                                  scale=convw[:, j, tap:tap + 1]),
                              reads=["ident", "convw"], writes=wr)
                    elif who == "dve":
                        T.add("dve", lambda e, dv3=dv3, j=j, tap=tap: e.tensor_scalar(out=dv3[:, tap, :], in0=ident[:],
                                                                                     scalar1=convw[:, j, tap:tap + 1], scalar2=None,
                                                                                     op0=ALU.mult),
                              reads=["ident", "convw"], writes=wr)
                    else:
                        T.add("pool", lambda e, dv3=dv3, j=j, tap=tap: e.tensor_scalar(out=dv3[:, tap, :], in0=ident[:],
                                                                                      scalar1=convw[:, j, tap:tap + 1], scalar2=1.0,
                                                                                      op0=ALU.mult, op1=ALU.mult),
                              reads=["ident", "convw"], writes=wr)
                prediag[j] = (hv, dv3, hk + [("pd", j, w) for w in ("act", "dve", "pool")])
                dma("sp", dscr[j], hv[:, 0:CW * 128], prediag[j][2], [("dscr", j)], "dscr%d" % (j % 2))
        s1, s1k = ps_next()
        ps_pinned.add(s1k[1])
        s2, s2k = ps_next()
        ps_pinned.add(s2k[1])
        def conv_chunk_slot(j, pc, pck):
            slot, wk = w_rr.next()
            dv3 = slot[:, 0:CW * 128].rearrange("p (t c) -> p t c", c=128)
            if s == 0:
                seen = set()
                for tap in range(CW):
                    who = (("dve", "act", "dve", "act", "dve", "pool", "dve", "act") if DIAG_DVE else ("act", "pool", "act", "act", "act", "pool", "act", "act"))[tap % 8]
                    first = who not in seen
                    seen.add(who)
                    wr = ([wk] if first else []) + [(wk, who)]
                    if who == "act":
                        T.add("act", lambda e, dv3=dv3, j=j, tap=tap: e.activation(out=dv3[:, tap, :], in_=ident[:], func=AF.Copy,
                                                                                  scale=convw[:, j, tap:tap + 1]),
                              reads=["ident", "convw"], writes=wr)
                    elif who == "dve":
                        T.add("dve", lambda e, dv3=dv3, j=j, tap=tap: e.tensor_scalar(out=dv3[:, tap, :], in0=ident[:],
                                                                                     scalar1=convw[:, j, tap:tap + 1], scalar2=None,
                                                                                     op0=ALU.mult),
                              reads=["ident", "convw"], writes=wr)
                    else:
                        T.add("pool", lambda e, dv3=dv3, j=j, tap=tap: e.tensor_scalar(out=dv3[:, tap, :], in0=ident[:],
                                                                                      scalar1=convw[:, j, tap:tap + 1], scalar2=1.0,
                                                                                      op0=ALU.mult, op1=ALU.mult),
                              reads=["ident", "convw"], writes=wr)
                    T.add("pe", lambda e, pc=pc, dv3=dv3, j=j, tap=tap: e.matmul(pc[:, :], lhsT=dv3[:, tap, :], rhs=hc[:, j, tap:tap + 512],
                                                                                start=(tap == 0), stop=(tap == CW - 1)),
                          reads=[wk, (wk, who), ("hc", j), ("hcm", j), "hc_hist"], writes=[pck])
                dma("sp", dscr[j], slot[:, 0:CW * 128], [wk, (wk, "act"), (wk, "dve"), (wk, "pool")], [("dscr", j)], "dscr%d" % (j % 2))
            else:
                dma("sp", slot[:, 0:CW * 128], dscr[j], [("dscr", j)], [wk], "wdg%d" % wk[1])

                def taps(e, pc=pc, dv3=dv3, j=j):
                    ins = None
                    for tap in range(CW):
                        ins = e.matmul(pc[:, :], lhsT=dv3[:, tap, :], rhs=hc[:, j, tap:tap + 512], start=(tap == 0), stop=(tap == CW - 1))
                    return ins
                T.add("pe", taps, reads=[wk, ("hc", j), ("hcm", j), "hc_hist"], writes=[pck])

        def conv_chunk(j):
            pc, pck = ps_next()
            if s == 0 and j in prediag:
                hv, dv3, pkeys = prediag[j]

                def taps0(e, pc=pc, dv3=dv3, j=j):
                    ins = None
                    for tap in range(CW):
                        ins = e.matmul(pc[:, :], lhsT=dv3[:, tap, :], rhs=hc[:, j, tap:tap + 512], start=(tap == 0), stop=(tap == CW - 1))
                    return ins
                T.add("pe", taps0, reads=pkeys + [("hc", j), ("hcm", j), "hc_hist"], writes=[pck])
            else:
                conv_chunk_slot(j, pc, pck)
            T.add("act", lambda e, pc=pc, j=j: e.activation(out=cs[:, j, :], in_=pc[:, :], func=AF.Identity,
                                                            bias=fmv[:, CONVB, j:j + 1]),
                  reads=[pck, "fmv"], writes=[("cs", j)])
            sq, sqk = sq_rr.next()
            T.add("dve", lambda e, sq=sq, j=j: e.tensor_tensor(out=sq[:], in0=cs[:, j, :], in1=cs[:, j, :], op=ALU.mult),
                  reads=[("cs", j)], writes=[sqk])
            T.add("pe", lambda e, j=j, s1=s1: e.matmul(s1[:, :], lhsT=ones_bf[:], rhs=cs[:, j, :], start=(j == 0), stop=(j == 7)),
                  reads=["ones", ("cs", j)], writes=[s1k])
            T.add("pe", lambda e, j=j, sq=sq, s2=s2: e.matmul(s2[:, :], lhsT=ones_bf[:], rhs=sq[:], start=(j == 0), stop=(j == 7)),
                  reads=["ones", sqk], writes=[s2k])

        if not CONV_IN_F:
            for j in range(8):
                conv_chunk(j)
        if s == 0:
            dbg("cspre0", cs[:], [128, 8, 512], BF16, [("cs", j) for j in range(8)])
        def emit_gate_b():
            for gi in range(2):
                slot, wk = load_cols(w_in_v, OFF_GB + 512 * gi)
                sv = v8(slot)
                for dd in range(4):
                    d = 4 * gi + dd
                    p, pk = ps_next()
                    mm_group(p[:, :], [(sv[:, k, dd * 128:(dd + 1) * 128], uT[:, k, :]) for k in range(8)], [wk] + uT_keys, pk)
                    T.add("act", lambda e, p=p, d=d: e.activation(out=cs[:, d, :], in_=p[:, :], func=AF.Sigmoid),
                          reads=[pk], writes=[("cs", d)])

        def emit_v(hd):
                slot, wk = load_cols(w_in_v, OFF_V + 512 * hd)
                sv = v8(slot)
                for t in range(4):
                    p, pk = ps_next()
                    mm_group(p[:, :], [(uT[:, k, 128 * t:128 * (t + 1)], sv[:, k, :]) for k in range(8)], [wk, ("uT", t)], pk)
                    T.add("act", lambda e, p=p, t=t: e.activation(out=vv[:, t, :], in_=p[:, :], func=AF.Copy), reads=[pk], writes=[("vv", t)])
                if s == 0:
                    p, pk = ps_next()
                    mm_group(p[0:NMETA, :], [(uTm[:, k, :], sv[:, k, :]) for k in range(8)], [wk, "uTm"], pk)
                    T.add("act", lambda e, p=p: e.activation(out=vm[:], in_=p[0:NMETA, :], func=AF.Copy), reads=[pk], writes=["vm"])

        def emit_sinit(hd):
            for c in range(2):
                p2, p2k = ps_next()
                mm_group(p2[:, :], [(kdm[:, c * 128:(c + 1) * 128], vm[:])], ["kdm", "vm"], p2k)
                T.add("dve", lambda e, p2=p2, c=c, hd=hd: e.tensor_copy(out=S[:, 2 * hd + c, :], in_=p2[:, :]),
                      reads=[p2k], writes=[("S", hd)])

        def emit_g(hd):
                slot, wk = load_cols(w_in_v, OFF_G + 512 * hd)
                sv = v8(slot)
                for t in range(4):
                    p, pk = ps_next()
                    mm_group(p[:, :], [(uT[:, k, 128 * t:128 * (t + 1)], sv[:, k, :]) for k in range(8)], [wk, ("uT", t)], pk)
                    T.add("act", lambda e, p=p, t=t: e.activation(out=gg[:, t, :], in_=p[:, :], func=AF.Silu), reads=[pk], writes=[("gg", t)])

        def emit_qkA(hd):
            def rotary(pa, pak, pb, pbk, o1, o2, ncol, ct, st, ckeys, okey):
                t1, t1k = tf_rr.next()
                t2, t2k = tf_rr.next()
                T.add("dve", lambda e: e.tensor_tensor(out=t1[:, 0:ncol], in0=pa, in1=ct, op=ALU.mult), reads=[pak] + ckeys, writes=[t1k])
                T.add("dve", lambda e: e.tensor_tensor(out=t2[:, 0:ncol], in0=pb, in1=st, op=ALU.mult), reads=[pbk] + ckeys, writes=[t2k])
                T.add(ROT_ENG, lambda e: e.tensor_tensor(out=o1, in0=t1[:, 0:ncol], in1=t2[:, 0:ncol], op=ALU.subtract),
                      reads=[t1k, t2k], writes=[okey])
                t3, t3k = tf_rr.next()
                t4, t4k = tf_rr.next()
                T.add("dve", lambda e: e.tensor_tensor(out=t3[:, 0:ncol], in0=pb, in1=ct, op=ALU.mult), reads=[pbk] + ckeys, writes=[t3k])
                T.add("dve", lambda e: e.tensor_tensor(out=t4[:, 0:ncol], in0=pa, in1=st, op=ALU.mult), reads=[pak] + ckeys, writes=[t4k])
                T.add(ROT_ENG, lambda e: e.tensor_tensor(out=o2, in0=t3[:, 0:ncol], in1=t4[:, 0:ncol], op=ALU.add),
                      reads=[t3k, t4k], writes=[okey])

            slot, wk = load_cols2(w_in_v, OFF_Q + 256 * hd, OFF_K + 256 * hd)
            sv = v8(slot)
            banks = []
            for c in range(2):
                p, pk = ps_next()
                mm_group(p[:, :], [(sv[:, k, c * 128:(c + 1) * 128], uT[:, k, :]) for k in range(8)], [wk] + uT_keys, pk)
                banks.append((p, pk))
            rotary(banks[0][0][:, :], banks[0][1], banks[1][0][:, :], banks[1][1], qk[:, 0, :], qk[:, 1, :], 512,
                   cos_sb[:], sin_sb[:], ["cos", "sin"], "qk_q")
            for c in range(2, 4):
                p, pk = ps_next()
                mm_group(p[:, :], [(sv[:, k, c * 128:(c + 1) * 128], uT[:, k, :]) for k in range(8)], [wk] + uT_keys, pk)
                banks.append((p, pk))
            if s == 0:
                pm, pmk = ps_next()
                for c in range(2):
                    mm_group(pm[:, 32 * c:32 * c + NMETA],
                             [(sv[:, k, (2 + c) * 128:(3 + c) * 128], uTm[:, k, :]) for k in range(8)], [wk, "uTm"], pmk)
            rotary(banks[2][0][:, :], banks[2][1], banks[3][0][:, :], banks[3][1], qk[:, 2, :], qk[:, 3, :], 512,
                   cos_sb[:], sin_sb[:], ["cos", "sin"], "qk_k")
            if s == 0:
                rotary(pm[:, 0:NMETA], pmk, pm[:, 32:32 + NMETA], pmk, kmeta[:, 0, :], kmeta[:, 1, :], NMETA,
                       cosm[:], sinm[:], ["cosm", "sinm"], "kmeta")
                pt, ptk = pst_rr.next()

                def trm(e, pt=pt):
                    ins = None
                    for c in range(2):
                        ins = e.transpose(pt[0:NMETA, c * 128:(c + 1) * 128], kmeta[:, c, :], ident[:])
                    return ins
                T.add("pe", trm, reads=["kmeta", "ident"], writes=[ptk])
                T.add("dve", lambda e, pt=pt, hd=hd: e.tensor_scalar(out=kdm[:], in0=pt[0:NMETA, 0:256], scalar1=kdecm[:, hd:hd + 1],
                                                                     scalar2=None, op0=ALU.mult),
                      reads=[ptk, "kdecm"], writes=["kdm"])

        def emit_qkB(hd):
            for t in range(4):
                T.add("dve", lambda e, hd=hd, t=t: e.scalar_tensor_tensor(
                    out=qp[:, :, 128 * t:128 * (t + 1)], in0=qk[:, 0:2, 128 * t:128 * (t + 1)], scalar=gpow(hd, 128 * t),
                    in1=qdec[:, hd, :].unsqueeze(1).to_broadcast([128, 2, 128]), op0=ALU.mult, op1=ALU.mult),
                    reads=["qk_q", "qdec"], writes=["qp"])
            for t in range(4):
                pt, ptk = pst_rr.next()

                def trk(e, pt=pt, t=t):
                    ins = None
                    for c in range(2):
                        ins = e.transpose(pt[:, c * 128:(c + 1) * 128], qk[:, 2 + c, 128 * t:128 * (t + 1)], ident[:])
                    return ins
                T.add("pe", trk, reads=["qk_k", "ident"], writes=[ptk])
                T.add("dve", lambda e, pt=pt, t=t, hd=hd: e.tensor_scalar(out=kd[:, t, :], in0=pt[:, 0:256], scalar1=kdec[:, hd:hd + 1],
                                                                          scalar2=gpow(hd, 384 - 128 * t), op0=ALU.mult, op1=ALU.mult),
                      reads=[ptk, "kdec"], writes=[("kd", t)])


        def emit_qk(hd):
            emit_qkA(hd)
            emit_qkB(hd)

        if PRE0 and not CONV_IN_F:
            if PREQK:
                emit_qk(0)
            emit_v(0)
            emit_g(0)
        def stage_DE():
            mu, muk = mu_b, "mu_b"
            T.add("act", lambda e, mu=mu, s1=s1: e.activation(out=mu, in_=s1[:, :], func=AF.Copy, scale=1.0 / D), reads=[s1k], writes=[muk])
            msq, msqk = rs_b, "rs_b"
            T.add("dve", lambda e, mu=mu, msq=msq: e.tensor_tensor(out=msq, in0=mu, in1=mu, op=ALU.mult), reads=[muk], writes=[msqk])
            T.add("dve", lambda e, msq=msq, s2=s2: e.scalar_tensor_tensor(out=msq, in0=s2[:, :], scalar=1.0 / D, in1=msq,
                                                          op0=ALU.mult, op1=ALU.subtract), reads=[s2k, msqk], writes=[msqk])
            T.add("dve", lambda e, msq=msq: e.tensor_scalar(out=msq, in0=msq, scalar1=EPS, scalar2=None, op0=ALU.add),
                  reads=[msqk], writes=[msqk])
            T.add("act", lambda e, msq=msq: e.activation(out=msq, in_=msq, func=AF.Sqrt), reads=[msqk], writes=[msqk])
            T.add("dve", lambda e, msq=msq: e.reciprocal(out=msq, in_=msq), reads=[msqk], writes=[msqk])
            ps_pinned.discard(s1k[1])
            ps_pinned.discard(s2k[1])
            for j in range(8):
                tf, tfk = tf_rr.next()
                T.add("dve", lambda e, tf=tf, j=j, mu=mu: e.tensor_tensor(out=tf[:], in0=cs[:, j, :], in1=mu, op=ALU.subtract),
                      reads=[("cs", j), muk], writes=[tfk])
                T.add("dve", lambda e, tf=tf, msq=msq: e.tensor_tensor(out=tf[:], in0=tf[:], in1=msq, op=ALU.mult),
                      reads=[tfk, msqk], writes=[tfk])
                T.add("act", lambda e, tf=tf, j=j: e.activation(out=cs[:, j, :], in_=tf[:], func=AF.Silu,
                                                                scale=fmv[:, LNG, j:j + 1], bias=fmv[:, LNB, j:j + 1]),
                      reads=[tfk, "fmv"], writes=[("cs", j)])
            cs_keys = [("cs", j) for j in range(8)]
            if s == 0:
                dbg("cs0", cs[:], [128, 8, 512], BF16, cs_keys)
            for gi in range(2):
                slot, wk = load_cols(w_in_v, OFF_GA + 512 * gi)
                sv = v8(slot)
                for dd in range(4):
                    d = 4 * gi + dd
                    p, pk = ps_next()
                    mm_group(p[:, :], [(sv[:, k, dd * 128:(dd + 1) * 128], uT[:, k, :]) for k in range(8)], [wk] + uT_keys, pk)
                    T.add("act", lambda e, p=p, d=d: e.activation(out=ycg[:, d, :], in_=p[:, :], func=AF.Sigmoid),
                          reads=[pk], writes=[("ycg", d)])
            if PREQKA and not CONV_IN_F:
                emit_qkA(0)
            for gi in range(2):
                slot, wk = load_cols(w_pw_v, 512 * gi)
                sv = v8(slot)
                for dd in range(4):
                    d = 4 * gi + dd
                    p, pk = ps_next()
                    mm_group(p[:, :], [(sv[:, k, dd * 128:(dd + 1) * 128], cs[:, k, :]) for k in range(8)], [wk] + cs_keys, pk)
                    T.add("dve", lambda e, p=p, d=d: e.tensor_tensor(out=ycg[:, d, :], in0=p[:, :], in1=ycg[:, d, :], op=ALU.mult),
                          reads=[pk, ("ycg", d)], writes=[("ycg", d)])

            if s == 0:
                dbg("ycgE0", ycg[:], [128, 8, 512], BF16, [("ycg", d) for d in range(8)])
        if not CONV_IN_F:
            stage_DE()
        for hd in range(4):
            if hd == 0 and PREQKA and not PREQK:
                emit_qkB(0)
            elif (hd == 0 and not PREQK) or (hd > 0 and not HOISTQK):
                emit_qk(hd)
            if (hd == 0 and not (PRE0 and not CONV_IN_F)) or (hd > 0 and not HOIST and not HOISTALL and not (HOISTQK and HOISTV)):
                emit_v(hd)
            if s == 0 and (hd == 0 or not HOISTALL):
                emit_sinit(hd)
            if not (PRE0 and not CONV_IN_F and hd == 0) and not (HOISTALL and hd > 0):
                emit_g(hd)
            if s == 0 and hd == 0:
                dbg("qk00", qk[:], [128, 4, 512], BF16, ["qk_q", "qk_k"])
                dbg("qp00", qp[:], [128, 2, 512], BF16, ["qp"])
                dbg("kd00", kd[:], [128, 4, 256], BF16, [("kd", t) for t in range(4)])
                dbg("vv00", vv[:], [128, 4, 512], BF16, [("vv", t) for t in range(4)])
                dbg("gg00", gg[:], [128, 4, 512], BF16, [("gg", t) for t in range(4)])
                dbg("S00", S[:, 0:2, :], [128, 2, 512], F32, [("S", 0)])
            T.add("act", lambda e, hd=hd: e.activation(out=Sbf[:], in_=S[:, 2 * hd:2 * hd + 2, :], func=AF.Copy),
                  reads=[("S", hd)], writes=["Sbf"])
            if CONV_IN_F:
                pkv = []
                for c in range(2):
                    p2, p2k = ps_next()
                    mm_group(p2[:, :], [(kd[:, t, c * 128:(c + 1) * 128], vv[:, t, :]) for t in range(4)],
                             [("kd", t) for t in range(4)] + [("vv", t) for t in range(4)], p2k)
                    pkv.append((p2, p2k))
                for c in range(2):
                    p2, p2k = pkv[c]
                    T.add("dve", lambda e, p2=p2, c=c, hd=hd: e.scalar_tensor_tensor(
                        out=S[:, 2 * hd + c, :], in0=S[:, 2 * hd + c, :], scalar=g512[hd], in1=p2[:, :], op0=ALU.mult, op1=ALU.add),
                        reads=[p2k, ("S", hd)], writes=[("S", hd)])
            pscs = []
            for nt in range(4):
                psc, psck = ps_next()
                for mt in range(nt + 1):
                    mm_group(psc[:, mt * 128:(mt + 1) * 128],
                             [(qk[:, 2 + c, 128 * mt:128 * (mt + 1)], qk[:, c, 128 * nt:128 * (nt + 1)]) for c in range(2)],
                             ["qk_q", "qk_k"], psck)
                pscs.append((psc, psck))
            scs = []
            for nt in range(4):
                psc, psck = pscs[nt]
                sc, sck = sc_all[:, SC_OFF[nt]:SC_OFF[nt] + (nt + 1) * 128], ("sc", nt)
                w = (nt + 1) * 128
                T.add("dve", lambda e, psc=psc, sc=sc, hd=hd, nt=nt, w=w: e.tensor_tensor(
                    out=sc[:, 0:w].rearrange("p (a b) -> p a b", b=128), in0=psc[:, 0:w].rearrange("p (a b) -> p a b", b=128),
                    in1=maskT[:, hd, 3 - nt:4, :], op=ALU.mult), reads=[psck, "maskT"], writes=[sck])
                scs.append((sc, sck))
            pos = []
            for nt in range(4):
                sc, sck = scs[nt]
                po, pok = ps_next()
                ps_pinned.add(pok[1])
                mm_group(po[:, :], [(sc[:, mt * 128:(mt + 1) * 128], vv[:, mt, :]) for mt in range(nt + 1)]
                         + [(qp[:, c, 128 * nt:128 * (nt + 1)], Sbf[:, c, :]) for c in range(2)],
                         [sck, "qp", "Sbf"] + [("vv", mt) for mt in range(nt + 1)], pok)
                pos.append((po, pok))
            if not CONV_IN_F:
                pkv = []
                for c in range(2):
                    p2, p2k = ps_next()
                    ps_pinned.add(p2k[1])
                    mm_group(p2[:, :], [(kd[:, t, c * 128:(c + 1) * 128], vv[:, t, :]) for t in range(4)],
                             [("kd", t) for t in range(4)] + [("vv", t) for t in range(4)], p2k)
                    pkv.append((p2, p2k))
            else:
                conv_chunk(2 * hd)
                conv_chunk(2 * hd + 1)
            if HOIST:
                if hd < 3:
                    emit_v(hd + 1)
                else:
                    emit_gate_b()
            for nt in range(4):
                po, pok = pos[nt]
                T.add("dve", lambda e, po=po, nt=nt: e.bn_stats(out=st6[:, nt, :], in_=po[:, :]), reads=[pok], writes=[("st6", nt)])
                T.add("dve", lambda e, nt=nt: e.bn_aggr(out=mv[:, nt, :], in_=st6[:, nt, :]), reads=[("st6", nt)], writes=[("mv", nt)])
            mv_keys = [("mv", nt) for nt in range(4)]
            vA, vK = sm(4)
            T.add("dve", lambda e, vA=vA: e.tensor_scalar(out=vA, in0=mv[:, :, 1], scalar1=EPS, scalar2=None, op0=ALU.add),
                  reads=mv_keys, writes=[vK])
            sA, sK = sm(4)
            T.add("act", lambda e, vA=vA, sA=sA: e.activation(out=sA, in_=vA, func=AF.Sqrt), reads=[vK], writes=[sK])
            rA, rK = sm(4)
            T.add("dve", lambda e, sA=sA, rA=rA: e.reciprocal(out=rA, in_=sA), reads=[sK], writes=[rK])
            if not CONV_IN_F:
                for c in range(2):
                    p2, p2k = pkv[c]
                    T.add("dve", lambda e, p2=p2, c=c, hd=hd: e.scalar_tensor_tensor(
                        out=S[:, 2 * hd + c, :], in0=S[:, 2 * hd + c, :], scalar=g512[hd], in1=p2[:, :], op0=ALU.mult, op1=ALU.add),
                        reads=[p2k, ("S", hd)], writes=[("S", hd)])
                    ps_pinned.discard(p2k[1])
            ogs = []
            for nt in range(4):
                po, pok = pos[nt]
                tf, tfk = tf_rr.next()
                T.add("dve", lambda e, po=po, tf=tf, nt=nt: e.scalar_tensor_tensor(out=tf[:], in0=po[:, :], scalar=mv[:, nt, 0:1],
                                                                                  in1=gg[:, nt, :], op0=ALU.subtract, op1=ALU.mult),
                      reads=[pok, ("mv", nt), ("gg", nt)], writes=[tfk])
                ps_pinned.discard(pok[1])
                og, ogk = og_rr.next()
                T.add("act", lambda e, tf=tf, og=og, rA=rA, nt=nt: e.activation(out=og[:], in_=tf[:], func=AF.Copy,
                                                                               scale=rA[:, nt:nt + 1]),
                      reads=[tfk, rK], writes=[ogk])
                ogs.append((og, ogk))
            if HOISTQK and hd < 3:
                if HOISTV:
                    emit_qkA(hd + 1)
                    emit_v(hd + 1)
                    emit_qkB(hd + 1)
                else:
                    emit_qk(hd + 1)
                if HOISTALL:
                    emit_v(hd + 1)
                    if s == 0:
                        emit_sinit(hd + 1)
                    emit_g(hd + 1)
            if hd == 3 and PREFA and s + 1 < NS:
                prefA_ctx[0] = rms_part1(a_items(s + 1), npre=2)
            if HOISTGB and hd == 3:
                emit_gate_b()
            for nt in range(4):
                og, ogk = ogs[nt]
                tc = slice(128 * nt, 128 * (nt + 1))
                pt, ptk = pst_rr.next()

                def tro(e, pt=pt, og=og):
                    ins = None
                    for c in range(4):
                        ins = e.transpose(pt[:, c * 128:(c + 1) * 128], og[:, c * 128:(c + 1) * 128], ident[:])
                    return ins
                T.add("pe", tro, reads=[ogk, "ident"], writes=[ptk])
                def evo(e, pt=pt, hd=hd, tc=tc):
                    ins = None
                    for c in range(4):
                        ins = e.activation(out=oT[:, 4 * hd + c, tc], in_=pt[:, c * 128:(c + 1) * 128], func=AF.Copy,
                                           scale=gn_fm[:, 4 * hd + c:4 * hd + c + 1])
                    return ins
                T.add("act", evo, reads=[ptk, "gn_fm"], writes=[("oT", hd, nt)])
        if CONV_IN_F:
            stage_DE()
        oT_keys = [("oT", hd, t) for hd in range(4) for t in range(4)]
        if s == 0:
            dbg("oT0", oT[:], [128, 16, 512], BF16, oT_keys)
        if not HOIST and not HOISTGB:
            emit_gate_b()
        if PREFA and s + 1 < NS:
            load_rot(s + 1)
            rms_part2(prefA_ctx[0], GMIX)
        for gi in range(4):
            slot, wk = wload([(lambda sl: v16(sl)[:, :, :], w_o_v[:, :, 256 * gi:256 * (gi + 1)])])
            sv = v16(slot)
            for dd in range(2):
                d = 2 * gi + dd
                p, pk = ps_next()
                mm_group(p[:, :], [(sv[:, k, dd * 128:(dd + 1) * 128], oT[:, k, :]) for k in range(16)], [wk] + oT_keys, pk)
                tf, tfk = tf_rr.next()
                T.add("dve", lambda e, p=p, tf=tf, d=d: e.tensor_tensor(out=tf[:], in0=p[:, :], in1=cs[:, d, :], op=ALU.mult),
                      reads=[pk, ("cs", d)], writes=[tfk])
                T.add("dve", lambda e, tf=tf, d=d: e.tensor_tensor(out=ycg[:, d, :], in0=tf[:], in1=ycg[:, d, :], op=ALU.add),
                      reads=[tfk, ("ycg", d)], writes=[("ycg", d)])
        ycg_keys = [("ycg", d) for d in range(8)]
        if s == 0:
            dbg("ycgG0", ycg[:], [128, 8, 512], BF16, ycg_keys)
        for gi in range(2):
            slot, wk = load_cols(w_out_v, 512 * gi)
            sv = v8(slot)
            for t, i in enumerate(tiles):
                p, pk = ps_next()
                mm_group(p[:, :], [(ycg[:, k, 128 * t:128 * (t + 1)], sv[:, k, :]) for k in range(8)], [wk] + ycg_keys, pk)
                T.add("dve", lambda e, p=p, i=i, gi=gi: e.tensor_tensor(out=h[:, i, 512 * gi:512 * (gi + 1)], in0=p[:, :],
                                                                        in1=h[:, i, 512 * gi:512 * (gi + 1)], op=ALU.add),
                      reads=[pk, ("h", i)], writes=[("h", i)])

    dbg("hmix", h[:], [128, NT, D], F32, [("h", i) for i in range(NT)])
    T.barrier()
    print("sbuf bytes remaining (mixer phase):", nc.sbuf_bytes_remaining() if callable(nc.sbuf_bytes_remaining) else nc.sbuf_bytes_remaining)
    mst.close()

    u2T = sb("u2T", [128, 8, SEQ], BF16)
    wg = [sb("wg%d" % i, [128, 8, 512], BF16) for i in range(2)]
    wu = [sb("wu%d" % i, [128, 8, 512], BF16) for i in range(2)]
    wd = [sb("wd%d" % i, [128, 4, D], BF16) for i in range(2)]
    hid_rr = RR("hid", [sb("hid%d" % i, [128, 4, 512], BF16) for i in range(2)])
    tfm_rr = RR("tfm", [sb("tfm%d" % i, [128, 512]) for i in range(3)])
    comb = sb("comb", [128, NT, 16])
    wrt = sb("wrt", [128, 8, 20], BF16)
    brt = sb("brt", [128, 20])
    lg = sb("lg", [128, NT, 20])
    gfin = sb("gfin", [128, D])
    ob_rr = RR("ob", [sb("ob%d" % i, [128, D]) for i in range(2)])
    cur["u_rr"] = RR("u_tmB", [sb("u_tmB%d" % i, [128, D], BF16) for i in range(2)])
    r4 = [sb("r4_%d" % i, [128, NT, 4]) for i in range(8)]
    r1 = [sb("r1_%d" % i, [128, NT]) for i in range(10)]

    dma("pool", wrt[:], w_rt_d.rearrange("(kc p) n -> p kc n", p=128), [], ["wrt"], "m0")
    dma("sp", brt[:], brt_d[:, :], [], ["brt"], "c0")
    dma("sp", gfin[:], gfin_d[:, :], [], ["gfin"], "c1")

    def load_expert(e):
        b = e % 2
        dma("pool", wg[b][:], w_g_d[e].rearrange("(kc p) n -> p kc n", p=128), [], [("wg", b)], "wg%d" % b)
        dma("pool", wu[b][:], w_u_d[e].rearrange("(kc p) n -> p kc n", p=128), [], [("wu", b)], "wu%d" % b)
        dma("pool", wd[b][:], w_d_d[e].rearrange("(fc p) n -> p fc n", p=128), [], [("wd", b)], "wd%d" % b)

    load_expert(0)
    load_expert(1)

    for i in range(NT):
        if i % 4 == 0:
            rms_batch([(h[:, ii, :], ("h", ii), (lambda ii=ii: u2T[:, :, 128 * ii:128 * (ii + 1)]), ("u2T", ii)) for ii in range(i, i + 4)], GFFN)
        p, pk = ps_next()
        mm_group(p[:, 0:20], [(u2T[:, k, 128 * i:128 * (i + 1)], wrt[:, k, :]) for k in range(8)], [("u2T", i), "wrt"], pk)
        T.add("dve", lambda e, p=p, i=i: e.tensor_tensor(out=lg[:, i, :], in0=p[:, 0:20], in1=brt[:], op=ALU.add),
              reads=[pk, "brt"], writes=["lg"])

    def bc4(a):
        return a.unsqueeze(2).to_broadcast([128, NT, 4])

    def dv(fn, reads, writes):
        return T.add("dve", fn, reads=reads, writes=writes)

    gl = lg[:, :, 0:4]
    gmax, gsum, pg, m1, m2, e21, w1p, w2p, tmp1, tmp2 = [t[:] for t in r1]
    ohg, ge, sel, selt, oh1, oh2, sel2, cl = [t[:] for t in r4]
    dv(lambda e: e.tensor_reduce(out=gmax, in_=gl, axis=AX.X, op=ALU.max), ["lg"], ["gmax"])
    dv(lambda e: e.tensor_tensor(out=ge, in0=gl, in1=bc4(gmax), op=ALU.subtract), ["lg", "gmax"], ["ge"])
    T.add("act", lambda e: e.activation(out=ge, in_=ge, func=AF.Exp), reads=["ge"], writes=["ge"])
    dv(lambda e: e.tensor_reduce(out=gsum, in_=ge, axis=AX.X, op=ALU.add), ["ge"], ["gsum"])
    dv(lambda e: e.reciprocal(out=pg, in_=gsum), ["gsum"], ["pg"])
    dv(lambda e: e.tensor_tensor(out=ohg, in0=gl, in1=bc4(gmax), op=ALU.is_equal), ["lg", "gmax"], ["ohg"])
    for g in range(4):
        dst = sel if g == 0 else selt
        dv(lambda e, g=g, dst=dst: e.tensor_tensor(out=dst, in0=lg[:, :, 4 + 4 * g:8 + 4 * g],
                                                   in1=ohg[:, :, g:g + 1].to_broadcast([128, NT, 4]), op=ALU.mult),
           ["lg", "ohg"], ["sel" if g == 0 else "selt"])
        if g > 0:
            dv(lambda e: e.tensor_tensor(out=sel, in0=sel, in1=selt, op=ALU.add), ["sel", "selt"], ["sel"])
    dv(lambda e: e.tensor_reduce(out=m1, in_=sel, axis=AX.X, op=ALU.max), ["sel"], ["m1"])
    dv(lambda e: e.tensor_tensor(out=oh1, in0=sel, in1=bc4(m1), op=ALU.is_equal), ["sel", "m1"], ["oh1"])
    dv(lambda e: e.scalar_tensor_tensor(out=sel2, in0=oh1, scalar=-1e30, in1=sel, op0=ALU.mult, op1=ALU.add),
       ["oh1", "sel"], ["sel2"])
    dv(lambda e: e.tensor_reduce(out=m2, in_=sel2, axis=AX.X, op=ALU.max), ["sel2"], ["m2"])
    dv(lambda e: e.tensor_tensor(out=oh2, in0=sel2, in1=bc4(m2), op=ALU.is_equal), ["sel2", "m2"], ["oh2"])
    dv(lambda e: e.tensor_tensor(out=e21, in0=m2, in1=m1, op=ALU.subtract), ["m1", "m2"], ["e21"])
    T.add("act", lambda e: e.activation(out=e21, in_=e21, func=AF.Exp), reads=["e21"], writes=["e21"])
    dv(lambda e: e.tensor_scalar(out=tmp1, in0=e21, scalar1=1.0, scalar2=None, op0=ALU.add), ["e21"], ["tmp1"])
    dv(lambda e: e.reciprocal(out=tmp2, in_=tmp1), ["tmp1"], ["tmp2"])
    dv(lambda e: e.tensor_tensor(out=w1p, in0=tmp2, in1=pg, op=ALU.mult), ["tmp2", "pg"], ["w1p"])
    dv(lambda e: e.tensor_tensor(out=w2p, in0=w1p, in1=e21, op=ALU.mult), ["w1p", "e21"], ["w2p"])
    dv(lambda e: e.tensor_tensor(out=cl, in0=oh1, in1=bc4(w1p), op=ALU.mult), ["oh1", "w1p"], ["cl"])
    dv(lambda e: e.tensor_tensor(out=selt, in0=oh2, in1=bc4(w2p), op=ALU.mult), ["oh2", "w2p"], ["selt"])
    dv(lambda e: e.tensor_tensor(out=cl, in0=cl, in1=selt, op=ALU.add), ["cl", "selt"], ["cl"])
    for g in range(4):
        dv(lambda e, g=g: e.tensor_tensor(out=comb[:, :, 4 * g:4 * g + 4], in0=cl,
                                          in1=ohg[:, :, g:g + 1].to_broadcast([128, NT, 4]), op=ALU.mult),
           ["cl", "ohg"], [("comb", g)])
    comb_keys = [("comb", g) for g in range(4)]
    dbg("u2T", u2T[:], [128, 8, SEQ], BF16, [("u2T", i) for i in range(NT)])
    dbg("lg", lg[:], [128, NT, 20], F32, ["lg"])
    dbg("comb", comb[:], [128, NT, 16], F32, comb_keys)

    out_ops = []

    def final_tiles(s):
        for i in range(4 * s, 4 * s + 4):
            junk, jk = cur["u_rr"].next()
            ssA, ssK = sm()
            T.add("act", lambda e, i=i, ssA=ssA, junk=junk: e.activation(out=junk[:], in_=h[:, i, :], func=AF.Square, accum_out=ssA),
                  reads=[("h", i)], writes=[ssK, jk])
            rsA, rsK = sm()
            T.add("dve", lambda e, ssA=ssA, rsA=rsA: e.tensor_scalar(out=rsA, in0=ssA, scalar1=1.0 / D, scalar2=EPS, op0=ALU.mult, op1=ALU.add),
                  reads=[ssK], writes=[rsK])
            sdA, sdK = sm()
            T.add("act", lambda e, rsA=rsA, sdA=sdA: e.activation(out=sdA, in_=rsA, func=AF.Sqrt), reads=[rsK], writes=[sdK])
            rA, rK = sm()
            T.add("dve", lambda e, sdA=sdA, rA=rA: e.reciprocal(out=rA, in_=sdA), reads=[sdK], writes=[rK])
            ob, obk = ob_rr.next()
            T.add("dve", lambda e, i=i, rA=rA, ob=ob: e.scalar_tensor_tensor(out=ob[:], in0=h[:, i, :], scalar=rA, in1=gfin[:],
                                                                            op0=ALU.mult, op1=ALU.mult),
                  reads=[("h", i), rK, "gfin"], writes=[obk])
            out_ops.append(dma("sp", out_d[128 * i:128 * (i + 1), :], ob[:], [obk], [("out", i)], "o%d" % (i % 2)))

    def gate_up(ex, s):
        b = ex % 2
        hid, hidk = hid_rr.next()
        u2_keys = [("u2T", 4 * s + t) for t in range(4)]
        for f in range(4):
            pgt, pgk = ps_next()
            mm_group(pgt[:, :], [(wg[b][:, k, f * 128:(f + 1) * 128], u2T[:, k, 512 * s:512 * (s + 1)]) for k in range(8)],
                     [("wg", b)] + u2_keys, pgk)
            put, puk = ps_next()
            mm_group(put[:, :], [(wu[b][:, k, f * 128:(f + 1) * 128], u2T[:, k, 512 * s:512 * (s + 1)]) for k in range(8)],
                     [("wu", b)] + u2_keys, puk)
            tf, tfk = tfm_rr.next()
            T.add("act", lambda e, pgt=pgt, tf=tf: e.activation(out=tf[:], in_=pgt[:, :], func=AF.Silu), reads=[pgk], writes=[tfk])
            T.add("dve", lambda e, put=put, tf=tf, hid=hid, f=f: e.tensor_tensor(out=hid[:, f, :], in0=put[:, :], in1=tf[:], op=ALU.mult),
                  reads=[puk, tfk], writes=[(hidk, f)])
        return hid, hidk

    def down(ex, s, hid, hidk):
        b = ex % 2
        hid_keys = [(hidk, f) for f in range(4)]
        for t in range(4):
            i = 4 * s + t
            for half in range(2):
                py, pyk = ps_next()
                mm_group(py[:, :], [(hid[:, f, 128 * t:128 * (t + 1)], wd[b][:, f, 512 * half:512 * (half + 1)]) for f in range(4)],
                         [("wd", b)] + hid_keys, pyk)
                T.add("dve", lambda e, py=py, i=i, half=half, ex=ex: e.scalar_tensor_tensor(
                    out=h[:, i, 512 * half:512 * (half + 1)], in0=py[:, :], scalar=comb[:, i, ex:ex + 1],
                    in1=h[:, i, 512 * half:512 * (half + 1)], op0=ALU.mult, op1=ALU.add),
                    reads=[pyk, ("h", i)] + comb_keys, writes=[("h", i)])
        if ex == NE - 1:
            final_tiles(s)
        if s == NS - 1 and ex + 2 < NE:
            load_expert(ex + 2)

    units = [(ex, s) for ex in range(NE) for s in range(NS)]
    if MOE_PIPE:
        prev = gate_up(*units[0])
        for u in range(len(units)):
            nxt = gate_up(*units[u + 1]) if u + 1 < len(units) else None
            down(units[u][0], units[u][1], *prev)
            prev = nxt
    else:
        for (ex, s) in units:
            hk = gate_up(ex, s)
            down(ex, s, *hk)

    T.add("sp", None, extra=out_ops + dbg_ops)

    with nc.Block() as block:
        T.emit(block)
    return nc


def _constants():
    f32 = np.float32
    half = 128
    inv = (f32(10000.0) ** (-(np.arange(half, dtype=f32)) / f32(half))).astype(f32)
    pos = np.arange(SEQ + NMETA, dtype=f32)
    ang = (pos[None, :] * inv[:, None]).astype(f32)
    cos_t = np.cos(ang).astype(f32)
    sin_t = np.sin(ang).astype(f32)
    lgam = np.log(f32(1.0) - f32(2.0) ** (-f32(5.0) - np.arange(4, dtype=f32))).astype(f32)
    m = np.arange(128, dtype=f32)[:, None]
    n = np.arange(128, dtype=f32)[None, :]
    same = (np.floor(m / 64) == np.floor(n / 64))
    causal_cross = (n >= 64) & (m < 64)
    maskT = np.zeros((128, 4, 4, 128), f32)
    qdec = np.zeros((128, 4, 128), f32)
    kdec = np.zeros((128, 4), f32)
    kdecm = np.zeros((NMETA, 4), f32)
    for hh in range(4):
        dec = np.exp(lgam[hh] * np.abs(n - m)).astype(f32)
        mk = np.where(same | causal_cross, dec, f32(0.0)).astype(f32)
        maskT[:, hh, 3, :] = mk * f32(1.0 / 16.0)
        for dd in range(1, 4):
            maskT[:, hh, 3 - dd, :] = np.exp(lgam[hh] * (f32(128.0 * dd) + n - m)).astype(f32) * f32(1.0 / 16.0)
        qdec[:, hh, :] = np.exp(lgam[hh] * (np.arange(128, dtype=f32) + 1.0)).astype(f32)[None, :]
        kdec[:, hh] = np.exp(lgam[hh] * (127.0 - np.arange(128, dtype=f32))).astype(f32) * f32(1.0 / 16.0)
        kdecm[:, hh] = np.exp(lgam[hh] * (15.0 - np.arange(NMETA, dtype=f32))).astype(f32) * f32(1.0 / 16.0)
    ident = np.eye(128, dtype=f32).astype(ml_dtypes.bfloat16)
    return dict(ident=ident, cos_t=cos_t, sin_t=sin_t, maskT=maskT, qdec=qdec, kdec=kdec, kdec_meta=kdecm)


_CACHE = {}


def kernel(x, meta_tokens, norm_mix_g, w_in, conv_dw_w, conv_dw_b, conv_ln_g, conv_ln_b,
           conv_pw_w, ret_gn_g, ret_w_o, w_out, norm_ffn_g, w_group_router, b_group_router,
           w_expert_router, b_expert_router, w_expert_gate, w_expert_up, w_expert_down,
           norm_final_g):
    f32 = np.float32
    A = lambda a: np.ascontiguousarray(np.asarray(a, dtype=f32))
    x = A(x)

    def fm(v):
        return np.asarray(v, f32).reshape(8, 128).T

    fmvec = np.zeros((128, 6, 8), f32)
    fmvec[:, 0] = fm(norm_mix_g[0])
    fmvec[:, 1] = fm(norm_ffn_g[0])
    fmvec[:, 2] = fm(conv_dw_b[0])
    fmvec[:, 3] = fm(conv_ln_g[0])
    fmvec[:, 4] = fm(conv_ln_b[0])
    convw_fm = A(np.asarray(conv_dw_w[0], f32).reshape(CW, 8, 128).transpose(2, 1, 0))
    gn_fm = A(np.asarray(ret_gn_g[0], f32).reshape(16, 128).T)
    w_rt = A(np.concatenate([np.asarray(w_group_router[0], f32), np.asarray(w_expert_router[0], f32)], axis=1))
    b_rt = A(np.broadcast_to(np.concatenate([np.asarray(b_group_router[0], f32), np.asarray(b_expert_router[0], f32)])[None, :], (128, 20)))
    gfin_b = A(np.broadcast_to(np.asarray(norm_final_g, f32)[None, :], (128, D)))
    shared = dict(
        meta=A(meta_tokens), w_in=A(w_in[0]), w_pw=A(conv_pw_w[0]), w_o=A(ret_w_o[0]), w_out=A(w_out[0]),
        w_rt=w_rt, w_gate=A(w_expert_gate[0]), w_up=A(w_expert_up[0]), w_down=A(w_expert_down[0]),
        fmvec=A(fmvec), convw_fm=convw_fm, gn_fm=gn_fm, b_rt=b_rt, gfin_b=gfin_b,
    )
    shared.update(_constants())
    if "nc" not in _CACHE:
        _CACHE["nc"] = build_program()
    nc = _CACHE["nc"]
    in_maps = []
    for b in range(8):
        m = dict(shared)
        m["x"] = np.ascontiguousarray(x[b])
        in_maps.append(m)
    res = run_bass_kernel_spmd(nc, in_maps, core_ids=list(range(8)))
    out = np.stack([np.asarray(res.results[b]["out"], dtype=f32) for b in range(8)], axis=0)
    return out
```
